# Optimizing a Trainium2 kernel written in Bass

```python
import jax, jax.numpy as jnp
from jax import lax
import numpy as np

D_MODEL = 2048
BATCH = 16
SEQ = 2048
DEPTH = 4

HEAD_DIM = 64
D_MIX = D_MODEL
D_RWKV = D_MIX // 2
D_SB = D_MIX - D_RWKV
H_RWKV = D_RWKV // HEAD_DIM
H_SB = D_SB // HEAD_DIM
D_FF = 4 * D_MODEL
W_LORA = 64
A_LORA = 64
V_LORA = 32
G_LORA = 160
P_R0 = 3 * D_RWKV + W_LORA + A_LORA + G_LORA
P_SB = 3 * D_SB
P_IN = P_R0 + P_SB
BLOCK_Q = 128
NORM_EPS = 1e-6
LNX_EPS = 64e-5

kernel_name = "hybrid_rwkv7_stickbreaking_sandwich"


def rmsnorm(x, g):
    xf = x.astype(jnp.float32)
    xf = xf * lax.rsqrt(jnp.mean(xf * xf, axis=-1, keepdims=True) + NORM_EPS)
    return (xf * g.astype(jnp.float32)).astype(x.dtype)


def token_shift(p, mu):
    prev = jnp.pad(p[:, :-1], ((0, 0), (1, 0), (0, 0)))
    return p + (prev - p) * mu


def rwkv7_scan(r, w, k, v, kk, a):
    B, T, H, N = r.shape
    seq = [jnp.moveaxis(t, 1, 0).astype(jnp.float32) for t in (r, w, k, v, kk, a)]

    def step(S, inp):
        r_t, w_t, k_t, v_t, kk_t, a_t = inp
        sa = jnp.einsum('bhvk,bhk->bhv', S, -kk_t)
        S = (S * w_t[:, :, None, :]
             + sa[..., None] * (kk_t * a_t)[:, :, None, :]
             + v_t[..., None] * k_t[:, :, None, :])
        y = jnp.einsum('bhvk,bhk->bhv', S, r_t)
        return S, y

    S0 = jnp.zeros((B, H, N, N), jnp.float32)
    _, ys = lax.scan(step, S0, tuple(seq))
    return jnp.moveaxis(ys, 0, 1)


def rwkv7_time_mix(p, p_vres, v_first, mu, mu_vres, w0, w_up, a0, a_up, g_up,
                   v0, v_up, k_k, k_a, r_k, lnx_w, lnx_b):
    B, T, _ = p.shape
    xs = token_shift(p, mu)
    c = np.cumsum([D_RWKV, D_RWKV, D_RWKV, W_LORA, A_LORA])
    r, k, v, dw, da, dg = jnp.split(xs, [int(i) for i in c], axis=-1)
    w = -jax.nn.softplus(-(w0 + jnp.tanh(dw) @ w_up)) - 0.5
    decay = jnp.exp(-jnp.exp(w))
    a = jax.nn.sigmoid(a0 + da @ a_up)
    g = jax.nn.sigmoid(dg) @ g_up
    if v_first is None:
        v_first = v
    else:
        xv = token_shift(p_vres, mu_vres)
        v = v + (v_first - v) * jax.nn.sigmoid(v0 + xv @ v_up)
    heads = lambda t: t.reshape(B, T, H_RWKV, HEAD_DIM)
    kk = heads(k * k_k)
    kk = kk * lax.rsqrt(jnp.maximum(jnp.sum(kk * kk, axis=-1, keepdims=True), 1e-24))
    k = k * (1 + (a - 1) * k_a)
    r_h, k_h, v_h = heads(r), heads(k), heads(v)
    y = rwkv7_scan(r_h, heads(decay), k_h, v_h, kk, heads(a))
    mean = jnp.mean(y, axis=-1, keepdims=True)
    var = jnp.mean(jnp.square(y - mean), axis=-1, keepdims=True)
    yn = ((y - mean) * lax.rsqrt(var + LNX_EPS)).reshape(B, T, D_RWKV)
    yn = (yn * lnx_w + lnx_b).astype(p.dtype)
    bonus = (jnp.sum(r_h * k_h * r_k, axis=-1, keepdims=True) * v_h).reshape(B, T, D_RWKV)
    return (yn + bonus) * g, v_first


def stick_breaking_attention(q, k, v):
    B, T, H, N = q.shape
    scale = 1.0 / np.sqrt(N)
    outs = []
    for i in range(T // BLOCK_Q):
        q_lo, q_hi = i * BLOCK_Q, (i + 1) * BLOCK_Q
        z = jnp.einsum('bqhd,bkhd->bhqk', q[:, q_lo:q_hi], k[:, :q_hi],
                       preferred_element_type=jnp.float32) * scale
        t_idx = q_lo + jnp.arange(BLOCK_Q)[:, None]
        s_idx = jnp.arange(q_hi)[None, :]
        causal = s_idx < t_idx
        log_not = jnp.where(causal, jax.nn.log_sigmoid(-z), 0.0)
        between = lax.cumsum(log_not, axis=3, reverse=True) - log_not
        att = jnp.where(causal, jnp.exp(jax.nn.log_sigmoid(z) + between), 0.0)
        outs.append(jnp.einsum('bhqk,bkhd->bqhd', att.astype(v.dtype), v[:, :q_hi]))
    return jnp.concatenate(outs, axis=1)


def setup_inputs(seed: int = 0) -> dict:
    key = jax.random.key(seed)
    ks = iter(jax.random.split(key, 32))
    nrm = lambda shape, s: jax.random.normal(next(ks), shape, jnp.float32) * s
    gain = lambda shape: 1.0 + nrm(shape, 0.05)
    L1 = DEPTH - 1
    return {
        "x": nrm((BATCH, SEQ, D_MODEL), 1.0),
        "pre_mix_g": gain((DEPTH, D_MODEL)),
        "post_mix_g": gain((DEPTH, D_MODEL)),
        "pre_mlp_g": gain((DEPTH, D_MODEL)),
        "post_mlp_g": gain((DEPTH, D_MODEL)),
        "w_in": nrm((DEPTH, D_MODEL, P_IN), D_MODEL ** -0.5),
        "w_in_vres": nrm((L1, D_MODEL, V_LORA), D_MODEL ** -0.5),
        "mu": jax.random.uniform(next(ks), (DEPTH, P_R0), jnp.float32),
        "mu_vres": jax.random.uniform(next(ks), (L1, V_LORA), jnp.float32),
        "w0": jax.random.uniform(next(ks), (DEPTH, D_RWKV), jnp.float32, -6.0, 1.0),
        "w_up": nrm((DEPTH, W_LORA, D_RWKV), 0.1 * W_LORA ** -0.5),
        "a0": nrm((DEPTH, D_RWKV), 0.5),
        "a_up": nrm((DEPTH, A_LORA, D_RWKV), 0.5 * A_LORA ** -0.5),
        "g_up": nrm((DEPTH, G_LORA, D_RWKV), G_LORA ** -0.5),
        "v0": nrm((L1, D_RWKV), 0.5),
        "v_up": nrm((L1, V_LORA, D_RWKV), 0.5 * V_LORA ** -0.5),
        "k_k": 0.85 + nrm((DEPTH, D_RWKV), 0.05),
        "k_a": 1.0 + nrm((DEPTH, D_RWKV), 0.05),
        "r_k": nrm((DEPTH, H_RWKV, HEAD_DIM), 0.1),
        "lnx_w": gain((DEPTH, D_RWKV)),
        "lnx_b": nrm((DEPTH, D_RWKV), 0.02),
        "sb_out_g": gain((DEPTH, D_SB)),
        "w_out": nrm((DEPTH, D_MIX, D_MODEL), D_MIX ** -0.5),
        "w_ff_up": nrm((DEPTH, D_MODEL, D_FF), D_MODEL ** -0.5),
        "w_ff_down": nrm((DEPTH, D_FF, D_MODEL), D_FF ** -0.5),
    }


def reference(x, pre_mix_g, post_mix_g, pre_mlp_g, post_mlp_g, w_in, w_in_vres, mu, mu_vres,
              w0, w_up, a0, a_up, g_up, v0, v_up, k_k, k_a, r_k, lnx_w, lnx_b,
              sb_out_g, w_out, w_ff_up, w_ff_down):
    B, T, _ = x.shape
    v_first = None
    for l in range(DEPTH):
        h = rmsnorm(x, pre_mix_g[l])
        if l == 0:
            w_cat = w_in[0]
        else:
            w_cat = jnp.concatenate([w_in[l], w_in_vres[l - 1]], axis=1)
        proj = jnp.einsum('btd,dp->btp', h, w_cat)
        p_r = proj[..., :P_R0]
        p_sb = proj[..., P_R0:P_IN]
        if l == 0:
            y_r, v_first = rwkv7_time_mix(p_r, None, None, mu[l], None, w0[l], w_up[l], a0[l],
                                          a_up[l], g_up[l], None, None, k_k[l], k_a[l],
                                          r_k[l], lnx_w[l], lnx_b[l])
        else:
            y_r, v_first = rwkv7_time_mix(p_r, proj[..., P_IN:], v_first, mu[l], mu_vres[l - 1],
                                          w0[l], w_up[l], a0[l], a_up[l], g_up[l],
                                          v0[l - 1], v_up[l - 1], k_k[l], k_a[l], r_k[l],
                                          lnx_w[l], lnx_b[l])
        q, k, v = jnp.split(p_sb, 3, axis=-1)
        heads = lambda t: t.reshape(B, T, H_SB, HEAD_DIM)
        y_s = stick_breaking_attention(heads(q), heads(k), heads(v))
        y_s = rmsnorm(y_s, sb_out_g[l].reshape(H_SB, HEAD_DIM)).reshape(B, T, D_SB)
        mix = jnp.einsum('btc,cd->btd', jnp.concatenate([y_r, y_s], axis=-1), w_out[l])
        x = x + rmsnorm(mix, post_mix_g[l])
        h = rmsnorm(x, pre_mlp_g[l])
        ff = jnp.square(jax.nn.relu(jnp.einsum('btd,df->btf', h, w_ff_up[l])))
        ff = jnp.einsum('btf,fd->btd', ff, w_ff_down[l])
        x = x + rmsnorm(ff, post_mlp_g[l])
    return x
```

```python
import numpy as np
import os
from contextlib import ExitStack
import concourse.bass as bass
import concourse.mybir as mybir
from concourse.bass_utils import run_bass_kernel_spmd

F32 = mybir.dt.float32
BF16 = mybir.dt.bfloat16
AF = mybir.ActivationFunctionType
ALU = mybir.AluOpType

D = 2048
DR = 1024
P_R0 = 3360
P_IN = 6432
DFF = 8192
LAM = float(np.exp(-0.5))
ENG = ['pe', 'act', 'dve', 'pool', 'sp']
SAME_SYNC = ('act', 'dve', 'pool')
SAME_ALL = bool(int(os.environ.get('SAME_ALL', '0')))

C_ID, C_BONES, C_STRICT, C_INCL, C_STRICTT, C_SBMASK, C_RESET = 0, 128, 256, 384, 768, 896, 1024
NCONST = 1024 + 512
V_PREMIX, V_POSTMIX, V_PREMLP, V_POSTMLP = 0, 16, 32, 48
V_MU = 64
V_MUV = 91
V_W0, V_A0, V_KK, V_KA, V_LNW, V_LNB, V_V0, V_RK, V_SBG = 92, 100, 108, 116, 124, 132, 140, 148, 156
NV = 164


def make_consts():
    c = np.zeros((128, NCONST), np.float32)
    p = np.arange(128)
    c[:, C_ID:C_ID + 128] = np.eye(128, dtype=np.float32)
    same = (p[:, None] // 64) == (p[None, :] // 64)
    c[:, C_BONES:C_BONES + 128] = same
    i = p[:, None] % 64
    t = p[None, :] % 64
    c[:, C_STRICT:C_STRICT + 128] = same & (i < t)
    c[:, C_INCL:C_INCL + 128] = same & (i <= t)
    c[:, C_STRICT + 256:C_STRICT + 384] = same & (i < t)
    c[:, C_INCL + 256:C_INCL + 384] = same & (i <= t)
    c[:, C_STRICTT:C_STRICTT + 128] = same & (i > t)
    c[:, C_SBMASK:C_SBMASK + 128] = (p[None, :] < p[:, None])
    c[:, C_RESET:C_RESET + 512] = (np.arange(512)[None, :] % 64 != 0)
    return c


def pack_vecs(inp, nl):
    v = np.zeros((128, nl * NV), np.float32)

    def put(l, col, arr, n):
        a = np.zeros(n * 128, np.float32)
        arr = np.asarray(arr, np.float32).reshape(-1)
        a[:arr.size] = arr
        v[:, l * NV + col:l * NV + col + n] = a.reshape(n, 128).T

    for l in range(nl):
        put(l, V_PREMIX, inp['pre_mix_g'][l], 16)
        put(l, V_POSTMIX, inp['post_mix_g'][l], 16)
        put(l, V_PREMLP, inp['pre_mlp_g'][l], 16)
        put(l, V_POSTMLP, inp['post_mlp_g'][l], 16)
        put(l, V_MU, inp['mu'][l], 27)
        put(l, V_W0, inp['w0'][l], 8)
        put(l, V_A0, inp['a0'][l], 8)
        put(l, V_KK, inp['k_k'][l], 8)
        put(l, V_KA, inp['k_a'][l], 8)
        put(l, V_LNW, inp['lnx_w'][l], 8)
        put(l, V_LNB, inp['lnx_b'][l], 8)
        put(l, V_RK, inp['r_k'][l], 8)
        put(l, V_SBG, inp['sb_out_g'][l], 8)
        if l > 0:
            put(l, V_MUV, inp['mu_vres'][l - 1], 1)
            put(l, V_V0, inp['v0'][l - 1], 8)
    return v


class Buf:
    __slots__ = ('name', 'w', 'r', 'chan', 'excl')

    def __init__(self, name, excl=False):
        self.name = name
        self.w = None
        self.r = {}
        self.chan = None
        self.excl = excl


class Prog:
    def __init__(self, nc):
        self.nc = nc
        self.ins = {e: [] for e in ENG}
        self.chan_count = []
        self.pending = {e: None for e in ENG}
        self.last_tok = {}
        self.free_ch = []
        self.scope_ch = None

    def _chan(self, buf):
        if buf.chan is None:
            if self.free_ch:
                buf.chan = self.free_ch.pop()
            else:
                buf.chan = len(self.chan_count)
                self.chan_count.append(0)
            if self.scope_ch is not None:
                self.scope_ch.append(buf.chan)
        return buf.chan

    def scope_begin(self):
        self.scope_ch = []

    def scope_end(self):
        self.free_ch.extend(self.scope_ch)
        self.scope_ch = None

    def op(self, eng, meth, reads=(), writes=(), chan_buf=None, same=False, **kw):
        lst = self.ins[eng]
        deps = set()
        if any(b.excl for b in reads):
            writes = list(writes) + [b for b in reads if b.excl and b not in writes]
            reads = [b for b in reads if not b.excl]
        for b in reads:
            if b.w is not None:
                deps.add(b.w)
        ch0 = self._chan(chan_buf) if chan_buf is not None else None
        for b in writes:
            if b.w is not None:
                if not (b is chan_buf and b.w[0] == 'c' and b.w[1] == ch0):
                    deps.add(b.w)
            deps.update(b.r.values())
        if self.pending[eng] is not None:
            deps |= self.pending[eng]
            self.pending[eng] = None
        if chan_buf is not None:
            ch = self._chan(chan_buf)
            self.chan_count[ch] += 1
            tok = ('c', ch, self.chan_count[ch])
            key = ('c', ch)
        else:
            ch = None
            tok = (eng, len(lst))
            key = eng
        lst.append({'fn': meth, 'kw': kw, 'deps': deps, 'chan': ch, 'signal': False, 'same': same})
        self.last_tok[key] = tok
        for b in reads:
            b.r[key] = tok
        for b in writes:
            b.w = tok
            b.r = {}
        return tok

    def dma(self, eng, out, in_, reads, writes, chan_buf):
        return self.op(eng, 'dma_start', reads, writes, chan_buf, out=out, in_=in_)

    def barrier(self):
        allt = set(self.last_tok.values())
        for e in ENG:
            self.pending[e] = set(allt) | (self.pending[e] or set())

    def finish(self):
        self.barrier()
        self.op('sp', None)

    def emit(self, es):
        nc = self.nc
        comp = ['pe', 'act', 'dve', 'pool']
        for e in ENG:
            for rec in self.ins[e]:
                for tok in rec['deps']:
                    if tok[0] == 'c':
                        continue
                    e2, i2 = tok
                    if e2 == e and not (e in SAME_SYNC and (rec['chan'] is not None or SAME_ALL or rec['same'])):
                        continue
                    self.ins[e2][i2]['signal'] = True
        for e in comp:
            cnt = 0
            for rec in self.ins[e]:
                if rec['signal']:
                    cnt += 1
                    rec['sigval'] = cnt
        esem = {e: es.enter_context(nc.semaphore('s_' + e)) for e in comp}
        csem = [es.enter_context(nc.semaphore('c_%d' % i)) for i in range(len(self.chan_count))]
        block = es.enter_context(nc.Block())
        prog = self

        def run(e, eo):
            known = {}
            for rec in prog.ins[e]:
                waits = {}
                for tok in rec['deps']:
                    if tok[0] == 'c':
                        key = ('c', tok[1])
                        val = 16 * tok[2]
                        sem = csem[tok[1]]
                    else:
                        e2, i2 = tok
                        if e2 == e and not (e in SAME_SYNC and (rec['chan'] is not None or SAME_ALL or rec['same'])):
                            continue
                        key = e2
                        val = prog.ins[e2][i2]['sigval']
                        sem = esem[e2]
                    if known.get(key, 0) >= val:
                        continue
                    if key not in waits or waits[key][0] < val:
                        waits[key] = (val, sem)
                for key, (val, sem) in waits.items():
                    known[key] = val
                    eo.wait_ge(sem, val)
                if rec['fn'] is None:
                    continue
                ins = getattr(eo, rec['fn'])(**rec['kw'])
                if rec['chan'] is not None:
                    ins.then_inc(csem[rec['chan']], 16)
                elif rec['signal']:
                    ins.then_inc(esem[e], 1)

        @block.tensor
        def _(eo):
            run('pe', eo)

        @block.scalar
        def _(eo):
            run('act', eo)

        @block.vector
        def _(eo):
            run('dve', eo)

        @block.gpsimd
        def _(eo):
            run('pool', eo)

        @block.sync
        def _(eo):
            run('sp', eo)


_UID = [0]


def _uid():
    _UID[0] += 1
    return _UID[0]


class Ring:
    def __init__(self, es, nc, name, shape, dtype, n):
        u = _uid()
        self.t = [es.enter_context(nc.sbuf_tensor('%s_%d_%d' % (name, u, i), shape, dtype)) for i in range(n)]
        self.b = [Buf('%s%d' % (name, i)) for i in range(n)]
        self.i = 0

    def next(self):
        k = self.i % len(self.t)
        self.i += 1
        return self.t[k], self.b[k]


class PsRing:
    def __init__(self, tens, bufs):
        self.t = tens
        self.b = bufs
        self.i = 0

    def next(self):
        k = self.i % len(self.t)
        self.i += 1
        return self.t[k], self.b[k]


def build(T, NL, NSEQ, debug=False, phases=None, rstop=99):
    assert T % 512 == 0
    NT = T // 512
    NQB = T // 128
    HT = min(1024, T)
    nc = bass.Bass("TRN2", target_bir_lowering=False)
    es = ExitStack()
    P = Prog(nc)

    def din(name, shape, dt=F32):
        return nc.dram_tensor(name, list(shape), dt, kind="ExternalInput").ap()

    def dscr(name, shape, dt=F32):
        kind = "ExternalOutput" if debug else "Internal"
        return nc.dram_tensor(name, list(shape), dt, kind=kind).ap()

    xT_in = din("xT", [NSEQ, D, T])
    consts_d = din("consts", [128, NCONST])
    vecs_d = din("vecs", [128, NL * NV])
    w_in = din("w_in", [NL, D, P_IN])
    w_in_vres = din("w_in_vres", [max(NL - 1, 1), D, 32])
    w_up = din("w_up", [NL, 64, DR])
    a_up = din("a_up", [NL, 64, DR])
    g_up = din("g_up", [NL, 160, DR])
    v_up = din("v_up", [max(NL - 1, 1), 32, DR])
    w_out = din("w_out", [NL, D, D])
    w_ff_up = din("w_ff_up", [NL, D, DFF])
    w_ff_down = din("w_ff_down", [NL, DFF, D])
    outT = nc.dram_tensor("outT", [NSEQ, D, T], F32, kind="ExternalOutput").ap()

    xT_s = dscr("xT_s", [NSEQ, D, T])
    oT_s = dscr("oT_s", [NSEQ, D, T])
    xsT_s = dscr("xsT_s", [3392, T])
    qT_s = dscr("qT_s", [1024, T], BF16)
    kT_s = dscr("kT_s", [1024, T], BF16)
    vtok_s = dscr("vtok_s", [T, 1024], BF16)
    mixT_s = dscr("mixT_s", [D, T], BF16)
    ffT_s = dscr("ffT_s", [DFF, T], BF16)
    vfT_s = dscr("vfT_s", [NSEQ, DR, T])

    dbufs = {}

    def DB(*key):
        if key not in dbufs:
            dbufs[key] = Buf(str(key))
        return dbufs[key]

    def sb(name, shape, dt, stack=None):
        return (stack or es).enter_context(nc.sbuf_tensor('%s_%d' % (name, _uid()), list(shape), dt))

    def ACT(out, in_, func, r, w, **kw):
        P.op('act', 'activation', r, w, out=out, in_=in_, func=func, **kw)

    def MM(out, lhsT, rhs, r, w, start=True, stop=True):
        P.op('pe', 'matmul', r, w, out=out, lhsT=lhsT, rhs=rhs, start=start, stop=stop)

    def TR(out, in_, r, w):
        P.op('pe', 'transpose', r + [cbfb], w, out=out, in_=in_, identity=IDENT)

    def TT(eng, out, in0, in1, op, r, w):
        P.op(eng, 'tensor_tensor', r, w, out=out, in0=in0, in1=in1, op=op)

    def TS(eng, out, in0, s1, s2, op0, op1, r, w):
        if s2 is None:
            P.op(eng, 'tensor_scalar', r, w, out=out, in0=in0, scalar1=s1, scalar2=None, op0=op0)
        else:
            P.op(eng, 'tensor_scalar', r, w, out=out, in0=in0, scalar1=s1, scalar2=s2, op0=op0, op1=op1)

    def STT(out, in0, scalar, in1, op0, op1, r, w):
        P.op('dve', 'scalar_tensor_tensor', r, w, out=out, in0=in0, scalar=scalar, in1=in1, op0=op0, op1=op1)

    def CPY(eng, out, in_, r, w):
        if eng == 'act':
            ACT(out, in_, AF.Copy, r, w)
        else:
            P.op(eng, 'tensor_copy', r, w, out=out, in_=in_)

    def RECIP(out, in_, r, w):
        P.op('dve', 'reciprocal', r, w, out=out, in_=in_)

    def MEMSET(eng, ap, val, w):
        P.op(eng, 'memset', [], w, ap=ap, constant=val)

    def SCAN(out, d0, d1, init, r, w):
        P.op('dve', 'tensor_tensor_scan', r, w, same=not isinstance(init, float), out=out, data0=d0, data1=d1,
             initial=init, op0=ALU.mult, op1=ALU.add)

    cf = sb("cf", [128, NCONST], F32)
    cfb = Buf("cf")
    vec = sb("vec", [128, NL * NV], F32)
    vecb = Buf("vec")
    omu = sb("omu", [128, NL * 28], F32)
    cbf = sb("cbf", [128, 4 * 128], BF16)
    cbfb = Buf("cbf")
    cst = sb("cst", [128, 8], F32)
    cstb = Buf("cst")
    ones_f = sb("ones_f", [128, 1024], F32)
    onesfb = Buf("onesf")

    P.dma('sp', cf[:], consts_d[:, :], [], [cfb], cfb)
    P.dma('sp', vec[:], vecs_d[:, :], [], [vecb], vecb)
    IDENT = cbf[:, 0:128]
    ONESB = cbf[:, 128:256]
    BONES = cbf[:, 256:384]
    BONES64 = cbf[:, 384:512]
    CPY('dve', cbf[:, 0:128], cf[:, C_ID:C_ID + 128], [cfb], [cbfb])
    MEMSET('dve', cbf[:, 128:256], 1.0, [cbfb])
    CPY('dve', cbf[:, 256:384], cf[:, C_BONES:C_BONES + 128], [cfb], [cbfb])
    TS('dve', cbf[:, 384:512], cf[:, C_BONES:C_BONES + 128], 1.0 / 64, None, ALU.mult, None, [cfb], [cbfb])
    MEMSET('dve', cst[:, 0:1], 1.0, [cstb])
    MEMSET('dve', cst[:, 1:2], 1e-6, [cstb])
    MEMSET('dve', cst[:, 2:3], 64e-5, [cstb])
    MEMSET('dve', ones_f[:], 1.0, [onesfb])
    for l in range(NL):
        TS('dve', omu[:, l * 28:l * 28 + 28], vec[:, l * NV + V_MU:l * NV + V_MU + 28], -1.0, 1.0, ALU.mult, ALU.add,
           [vecb], [vecb])
    C_ONE = cst[:, 0:1]
    C_EPS6 = cst[:, 1:2]
    C_EPSLN = cst[:, 2:3]
    MSK4 = cf[:, C_STRICT:C_STRICT + 512]
    MSKT = cf[:, C_STRICTT:C_STRICTT + 128]
    SBM = cf[:, C_SBMASK:C_SBMASK + 128]
    RESET = cf[:, C_RESET:C_RESET + 512]

    def V(l, col, n=1):
        return vec[:, l * NV + col:l * NV + col + n]

    pst = [es.enter_context(nc.psum_tensor('ps%d' % i, [128, 512], F32)) for i in range(8)]
    psb = [Buf('ps%d' % i, excl=True) for i in range(8)]

    def xview(ap2d):
        return ap2d.rearrange("(kc p) t -> p kc t", p=128)

    def c3(ap):
        return ap.rearrange("p (n c) -> p n c", c=64)

    NW = 256

    def rms_rstd(src3, srcb, sqr, rsr, psr):
        ps, pb = psr.next()
        for kc in range(16):
            sq, sqb = sqr.next()
            ACT(sq[:], src3[:, kc, :], AF.Square, [srcb], [sqb])
            MM(ps[:, 0:NW], ONESB, sq[:], [sqb, cbfb], [pb], start=(kc == 0), stop=(kc == 15))
        rs, rsb = rsr.next()
        ACT(rs[:], ps[:, 0:NW], AF.Sqrt, [pb, cstb], [rsb], bias=C_EPS6, scale=1.0 / D)
        RECIP(rs[:], rs[:], [rsb], [rsb])
        return rs, rsb

    def norm_pass(st, l_post, gpost_col, l_pre, gpre_col, src_x, src_key, o_ap, dst_x, dst_key, hT, hTb, o_key=None):
        xr = Ring(st, nc, "np_x", [128, 16, NW], F32, 2)
        orr = Ring(st, nc, "np_o", [128, 16, NW], F32, 2) if o_ap is not None else None
        sqr = Ring(st, nc, "np_sq", [128, NW], BF16, 4)
        rsr = Ring(st, nc, "np_rs", [128, NW], F32, 3)
        psr = PsRing(pst[0:2], psb[0:2])
        for t2 in range(T // NW):
            tt = (t2 * NW) // 512
            ts = slice(t2 * NW, (t2 + 1) * NW)
            xt, xb = xr.next()
            for q4 in range(2):
                P.dma('sp', xt[:, q4 * 8:(q4 + 1) * 8, :], xview(src_x)[:, q4 * 8:(q4 + 1) * 8, ts],
                      [DB(src_key, tt)], [xb], xb)
            if o_ap is not None:
                ot, ob = orr.next()
                for q4 in range(2):
                    P.dma('act', ot[:, q4 * 8:(q4 + 1) * 8, :], xview(o_ap)[:, q4 * 8:(q4 + 1) * 8, ts],
                          [DB(o_key, tt)], [ob], ob)
                rs, rsb = rms_rstd(ot, ob, sqr, rsr, psr)
                for kc in range(16):
                    STT(ot[:, kc, :], ot[:, kc, :], V(l_post, gpost_col + kc), rs[:], ALU.mult, ALU.mult,
                        [ob, rsb, vecb], [ob])
                    TT('pool', xt[:, kc, :], xt[:, kc, :], ot[:, kc, :], ALU.add, [ob, xb], [xb])
                if dst_x is not None:
                    for q4 in range(2):
                        P.dma('sp', xview(dst_x)[:, q4 * 8:(q4 + 1) * 8, ts], xt[:, q4 * 8:(q4 + 1) * 8, :],
                              [xb], [DB(dst_key, tt)], xb)
            if hT is not None:
                rs, rsb = rms_rstd(xt, xb, sqr, rsr, psr)
                for kc in range(16):
                    STT(hT[:, kc, ts], xt[:, kc, :], V(l_pre, gpre_col + kc), rs[:], ALU.mult, ALU.mult,
                        [xb, rsb, vecb], [hTb])

    def dense_fm(wr, rhsT, rhsb, groups, evac):
        psr = PsRing(pst[2:8], psb[2:8])
        for (W, f0, n, chunks) in groups:
            wt, wb = wr.next()
            Wv = W.rearrange("(kc p) f -> p kc f", p=128)
            for q4 in range(4):
                P.dma('pool', wt[:, q4 * 4:(q4 + 1) * 4, 0:n], Wv[:, q4 * 4:(q4 + 1) * 4, f0:f0 + n], [], [wb], wb)
            for (coff, fsz, info) in chunks:
                for tt in range(NT):
                    ps, pb = psr.next()
                    for kc in range(16):
                        MM(ps[0:fsz, :], wt[:, kc, coff:coff + fsz], rhsT[:, kc, tt * 512:(tt + 1) * 512],
                           [wb, rhsb], [pb], start=(kc == 0), stop=(kc == 15))
                    evac(info, fsz, tt, ps, pb)

    def phase_A(st, l, s, hT, hTb):
        wr = Ring(st, nc, "A_w", [128, 16, 512], BF16, 2)
        stA = Ring(st, nc, "A_a", [128, T], F32, 2)
        stB = Ring(st, nc, "A_b", [128, T + 1], F32, 2)
        stQ = Ring(st, nc, "A_q", [128, T], BF16, 2)
        stV = Ring(st, nc, "A_v", [128, 512], BF16, 3)
        for t_, b_ in zip(stB.t, stB.b):
            MEMSET('pool', t_[:, 0:1], 0.0, [b_])
        cur = {}
        W = w_in[l]

        def evac(info, fsz, tt, ps, pb):
            kind, idx = info
            ts = slice(tt * 512, (tt + 1) * 512)
            if kind in ('r', 'vres'):
                if tt == 0:
                    cur['A'] = stA.next()
                    cur['B'] = stB.next()
                (At, Ab), (Bt, Bb) = cur['A'], cur['B']
                if kind == 'r':
                    mu_ap = V(l, V_MU + idx)
                    omu_ap = omu[:, l * 28 + idx:l * 28 + idx + 1]
                    row0 = idx * 128
                else:
                    mu_ap = V(l, V_MUV)
                    omu_ap = omu[:, l * 28 + 27:l * 28 + 28]
                    row0 = P_R0
                ACT(At[0:fsz, ts], ps[0:fsz, :], AF.Copy, [pb, vecb], [Ab], scale=omu_ap[0:fsz, :])
                ACT(Bt[0:fsz, 1 + tt * 512:1 + (tt + 1) * 512], ps[0:fsz, :], AF.Copy, [pb, vecb], [Bb],
                    scale=mu_ap[0:fsz, :])
                if tt == NT - 1:
                    TT('dve', At[0:fsz, :], At[0:fsz, :], Bt[0:fsz, 0:T], ALU.add, [Ab, Bb], [Ab])
                    P.dma('sp', xsT_s[row0:row0 + fsz, :], At[0:fsz, :], [Ab], [DB('xsT', row0 // 128)], Ab)
            else:
                if tt == 0:
                    cur['Q'] = stQ.next()
                Qt, Qb = cur['Q']
                sc = 0.125 if kind == 'q' else 1.0
                ACT(Qt[:, ts], ps[:], AF.Copy, [pb], [Qb], scale=sc)
                if tt == NT - 1:
                    dst = qT_s if kind == 'q' else kT_s
                    P.dma('sp', dst[idx * 128:(idx + 1) * 128, :], Qt[:], [Qb], [DB(kind + 'T', idx)], Qb)

        groups = []
        for g in range(6):
            groups.append((W, g * 512, 512, [(c * 128, 128, ('r', g * 4 + c)) for c in range(4)]))
        groups.append((W, 3072, 288, [(0, 128, ('r', 24)), (128, 128, ('r', 25)), (256, 32, ('r', 26))]))
        for g in range(2):
            groups.append((W, P_R0 + g * 512, 512, [(c * 128, 128, ('q', g * 4 + c)) for c in range(4)]))
        for g in range(2):
            groups.append((W, P_R0 + 1024 + g * 512, 512, [(c * 128, 128, ('k', g * 4 + c)) for c in range(4)]))
        if l > 0:
            groups.append((w_in_vres[l - 1], 0, 32, [(0, 32, ('vres', 0))]))
        dense_fm(wr, hT, hTb, groups, evac)

        psr = PsRing(pst[2:8], psb[2:8])
        Wv = W.rearrange("(kc p) f -> p kc f", p=128)
        for g in range(2):
            wt, wb = wr.next()
            f0 = P_R0 + 2048 + g * 512
            for q4 in range(4):
                P.dma('pool', wt[:, q4 * 4:(q4 + 1) * 4, :], Wv[:, q4 * 4:(q4 + 1) * 4, f0:f0 + 512], [], [wb], wb)
            for tb in range(NQB):
                ps, pb = psr.next()
                for kc in range(16):
                    MM(ps[:], hT[:, kc, tb * 128:(tb + 1) * 128], wt[:, kc, :], [wb, hTb], [pb],
                       start=(kc == 0), stop=(kc == 15))
                vt, vb = stV.next()
                ACT(vt[:], ps[:], AF.Copy, [pb], [vb])
                P.dma('sp', vtok_s[tb * 128:(tb + 1) * 128, g * 512:(g + 1) * 512], vt[:], [vb],
                      [DB('vtok', tb // 4)], vb)

    def load_rhsT(hT, hTb, src, key):
        for q4 in range(4):
            for tt in range(NT):
                P.dma('sp', hT[:, q4 * 4:(q4 + 1) * 4, tt * 512:(tt + 1) * 512],
                      xview(src)[:, q4 * 4:(q4 + 1) * 4, tt * 512:(tt + 1) * 512],
                      [DB(key, kc) for kc in range(q4 * 4, q4 * 4 + 4)], [hTb], hTb)

    def phase_C(st, l, s, hT, hTb):
        wr = Ring(st, nc, "C_w", [128, 16, 512], BF16, 2)
        stO = Ring(st, nc, "C_o", [128, T], F32, 2)
        cur = {}

        def evac(info, fsz, tt, ps, pb):
            if tt == 0:
                cur['O'] = stO.next()
            Ot, Ob = cur['O']
            ACT(Ot[:, tt * 512:(tt + 1) * 512], ps[:], AF.Copy, [pb], [Ob])
            if tt == NT - 1:
                P.dma('sp', oT_s[s][info * 128:(info + 1) * 128, :], Ot[:], [Ob], [DB(('oT', s), t_) for t_ in range(NT)], Ob)

        groups = [(w_out[l], g * 512, 512, [(c * 128, 128, g * 4 + c) for c in range(4)]) for g in range(4)]
        dense_fm(wr, hT, hTb, groups, evac)

    def phase_F1(st, l, hT, hTb):
        wr = Ring(st, nc, "F1_w", [128, 16, 512], BF16, 2)
        stR = Ring(st, nc, "F_r", [128, 512], F32, 3)
        stF = Ring(st, nc, "F_f", [128, T], BF16, 2)
        cur = {}

        def evac(info, fsz, tt, ps, pb):
            if tt == 0:
                cur['F'] = stF.next()
            Ft, Fb = cur['F']
            rt, rb = stR.next()
            ACT(rt[:], ps[:], AF.Relu, [pb], [rb])
            if tt % 2 == 0:
                TT('dve', Ft[:, tt * 512:(tt + 1) * 512], rt[:], rt[:], ALU.mult, [rb], [Fb])
            else:
                ACT(Ft[:, tt * 512:(tt + 1) * 512], rt[:], AF.Square, [rb], [Fb])
            if tt == NT - 1:
                P.dma('sp', ffT_s[info * 128:(info + 1) * 128, :], Ft[:], [Fb], [DB('ffT', info)], Fb)

        groups = [(w_ff_up[l], g * 512, 512, [(c * 128, 128, g * 4 + c) for c in range(4)]) for g in range(16)]
        dense_fm(wr, hT, hTb, groups, evac)

    def phase_F2(st, l, s):
        ffh = sb("F2_ff", [128, 64, HT], BF16, st)
        ffq = [Buf("F2_ff%d" % q) for q in range(4)]
        wr = Ring(st, nc, "F2_w", [128, 64, 128], BF16, 2)
        stO = Ring(st, nc, "F2_o", [128, HT], F32, 2)
        NJ = HT // 512
        bi = 0
        Wv = w_ff_down[l].rearrange("(kc p) f -> p kc f", p=128)
        fv = ffT_s.rearrange("(kc p) t -> p kc t", p=128)
        for half in range(T // HT):
            hs = slice(half * HT, (half + 1) * HT)
            for q in range(16):
                P.dma('sp' if q % 2 == 0 else 'act', ffh[:, q * 4:(q + 1) * 4, :], fv[:, q * 4:(q + 1) * 4, hs],
                      [DB('ffT', kc) for kc in range(q * 4, q * 4 + 4)], [ffq[q // 4]], ffq[q // 4])
            for fc in range(16):
                wt, wb = wr.next()
                for q in range(4):
                    P.dma('pool', wt[:, q * 16:(q + 1) * 16, :], Wv[:, q * 16:(q + 1) * 16, fc * 128:(fc + 1) * 128],
                          [], [wb], wb)
                pss = []
                for j in range(NJ):
                    pss.append((pst[bi % 8], psb[bi % 8]))
                    bi += 1
                for kc in range(64):
                    for j in range(NJ):
                        ps, pb = pss[j]
                        MM(ps[:], wt[:, kc, :], ffh[:, kc, j * 512:(j + 1) * 512], [wb, ffq[kc // 16]], [pb],
                           start=(kc == 0), stop=(kc == 63))
                Ot, Ob = stO.next()
                for j in range(NJ):
                    ps, pb = pss[j]
                    ACT(Ot[:, j * 512:(j + 1) * 512], ps[:], AF.Copy, [pb], [Ob])
                P.dma('sp', oT_s[s][fc * 128:(fc + 1) * 128, hs], Ot[:], [Ob],
                      [DB(('oT', s), t_) for t_ in range(half * NJ, (half + 1) * NJ)], Ob)

    def phase_S(st, l):
        q2r = Ring(st, nc, "S_q", [128, T], BF16, 2)
        k2r = Ring(st, nc, "S_k", [128, T], BF16, 2)
        v2r = Ring(st, nc, "S_v", [128, NQB, 128], BF16, 2)
        er = Ring(st, nc, "S_e", [128, T], F32, 5)
        spr = Ring(st, nc, "S_sp", [128, 1024], F32, 6)
        psr_ = Ring(st, nc, "S_ps", [128, T + 1], F32, 4)
        attr = Ring(st, nc, "S_att", [128, T], BF16, 3)
        attTr = Ring(st, nc, "S_attT", [128, NQB, 128], BF16, 3)
        ntr = Ring(st, nc, "S_nt", [128, 1], F32, 6)
        o2r = Ring(st, nc, "S_o2", [128, 128], BF16, 2)
        oTr = Ring(st, nc, "S_oT", [128, T], BF16, 2)
        sqr = Ring(st, nc, "S_sq", [128, 512], BF16, 2)
        rsr = Ring(st, nc, "S_rs", [128, 512], F32, 2)
        yr = Ring(st, nc, "S_y", [128, 512], BF16, 2)
        vv = vtok_s.rearrange("(sb s) c -> s sb c", s=128)
        cnt = {'z': 0, 't': 0, 'e': 0}
        hpd = {}

        def zpair():
            k = (cnt['z'] % 2) * 2
            cnt['z'] += 1
            return k

        def tbank():
            k = 4 + (cnt['t'] % 2)
            cnt['t'] += 1
            return k

        def segs(nk):
            return [(c0, min(1024, nk - c0)) for c0 in range(0, nk, 1024)]

        def s0(u):
            hp, tb, j = u['hp'], u['tb'], u['j']
            if tb == 0 and j == 0:
                q2, q2b = q2r.next()
                k2, k2b = k2r.next()
                v2, v2b = v2r.next()
                P.dma('sp', q2[:], qT_s[hp * 128:(hp + 1) * 128, :], [DB('qT', hp)], [q2b], q2b)
                P.dma('sp', k2[:], kT_s[hp * 128:(hp + 1) * 128, :], [DB('kT', hp)], [k2b], k2b)
                P.dma('act', v2[:], vv[:, :, hp * 128:(hp + 1) * 128], [DB('vtok', i) for i in range((NQB + 3) // 4)],
                      [v2b], v2b)
                oT, oTb = oTr.next()
                hpd[hp] = dict(q2=q2, q2b=q2b, k2=k2, k2b=k2b, v2=v2, v2b=v2b, oT=oT, oTb=oTb)
            h = hpd[hp]
            nk = (tb + 1) * 128
            u['nk'] = nk
            pbs = slice(64 * j, 64 * j + 64)
            u['z'] = []
            for (c0, n) in segs(nk):
                zb = zpair()
                for hb in range((n + 511) // 512):
                    nn = min(512, n - hb * 512)
                    MM(pst[zb + hb][:, 0:nn], h['q2'][pbs, tb * 128:(tb + 1) * 128],
                       h['k2'][pbs, c0 + hb * 512:c0 + hb * 512 + nn], [h['q2b'], h['k2b']], [psb[zb + hb]])
                u['z'].append(zb)

        def s1(u):
            nk = u['nk']
            E, Eb = er.next()
            u['E'], u['Eb'] = E, Eb
            u['SP'] = []
            for si, (c0, n) in enumerate(segs(nk)):
                zb = u['z'][si]
                for hb in range((n + 511) // 512):
                    nn = min(512, n - hb * 512)
                    ACT(E[:, c0 + hb * 512:c0 + hb * 512 + nn], pst[zb + hb][:, 0:nn], AF.Exp, [psb[zb + hb]], [Eb])
                SP, SPb = spr.next()
                ACT(SP[:, 0:n], E[:, c0:c0 + n], AF.Ln, [Eb, cstb], [SPb], bias=C_ONE, scale=1.0)
                u['SP'].append((SP, SPb))

        def s2(u):
            nk = u['nk']
            PS, PSb = psr_.next()
            u['PS'], u['PSb'] = PS, PSb
            MEMSET('pool', PS[:, 0:1], 0.0, [PSb])
            for si, (c0, n) in enumerate(segs(nk)):
                SP, SPb = u['SP'][si]
                if c0 + n == nk:
                    TT('pool', SP[:, n - 128:n], SP[:, n - 128:n], SBM, ALU.mult, [SPb, cfb], [SPb])
                SCAN(PS[:, 1 + c0:1 + c0 + n], ones_f[:, 0:n], SP[:, 0:n],
                     (PS[:, c0:c0 + 1] if c0 > 0 else 0.0), [SPb, onesfb, PSb], [PSb])

        def s3(u):
            nk = u['nk']
            PS, PSb = u['PS'], u['PSb']
            NTt, NTb = ntr.next()
            TS('pool', NTt[:], PS[:, nk:nk + 1], -1.0, None, ALU.mult, None, [PSb], [NTb])
            for (c0, n) in segs(nk):
                ACT(PS[:, c0:c0 + n], PS[:, c0:c0 + n], AF.Exp, [PSb, NTb], [PSb], bias=NTt[:], scale=1.0)

        def s4(u):
            nk = u['nk']
            ATT, ATTb = attr.next()
            u['ATT'], u['ATTb'] = ATT, ATTb
            cnt['e'] += 1
            nA = (nk // 256) * 128
            if nA > 0:
                TT('pool', ATT[:, 0:nA], u['E'][:, 0:nA], u['PS'][:, 0:nA], ALU.mult, [u['Eb'], u['PSb']], [ATTb])
            TT('dve', ATT[:, nA:nk], u['E'][:, nA:nk], u['PS'][:, nA:nk], ALU.mult, [u['Eb'], u['PSb']], [ATTb])
            TT('pool', ATT[:, nk - 128:nk], ATT[:, nk - 128:nk], SBM, ALU.mult, [ATTb, cfb], [ATTb])

        def s5(u):
            tb = u['tb']
            ATT, ATTb = u['ATT'], u['ATTb']
            ATTT, ATTTb = attTr.next()
            u['ATTT'], u['ATTTb'] = ATTT, ATTTb
            for g8 in range((tb + 8) // 8):
                nb = min(8, tb + 1 - g8 * 8)
                tk = tbank()
                pT = pst[tk][:].bitcast(BF16)
                for s8 in range(nb):
                    sbk = g8 * 8 + s8
                    TR(pT[:, s8 * 128:(s8 + 1) * 128], ATT[:, sbk * 128:(sbk + 1) * 128], [ATTb], [psb[tk]])
                CPY('act', ATTT[:, g8 * 8:g8 * 8 + nb, :],
                    pT[:, 0:nb * 128].rearrange("p (a b) -> p a b", b=128), [psb[tk]], [ATTTb])

        def s6(u):
            hp, tb, j = u['hp'], u['tb'], u['j']
            h = hpd[hp]
            po, pob = pst[6 + (tb % 2)], psb[6 + (tb % 2)]
            for sbk in range(tb + 1):
                MM(po[:, j * 64:(j + 1) * 64], u['ATTT'][:, sbk, :], h['v2'][:, sbk, j * 64:(j + 1) * 64],
                   [u['ATTTb'], h['v2b']], [pob], start=(sbk == 0), stop=(sbk == tb))

        def s7(u):
            hp, tb, j = u['hp'], u['tb'], u['j']
            if j == 0:
                return
            h = hpd[hp]
            oT, oTb = h['oT'], h['oTb']
            po, pob = pst[6 + (tb % 2)], psb[6 + (tb % 2)]
            O2, O2b = o2r.next()
            ACT(O2[:], po[:, 0:128], AF.Copy, [pob], [O2b])
            tk = tbank()
            pT2 = pst[tk][:].bitcast(BF16)
            TR(pT2[:, 0:128], O2[:], [O2b], [psb[tk]])
            CPY('dve', oT[:, tb * 128:(tb + 1) * 128], pT2[:, 0:128], [psb[tk]], [oTb])
            if tb == NQB - 1:
                for tt in range(NT):
                    ts = slice(tt * 512, (tt + 1) * 512)
                    sq, sqb = sqr.next()
                    ACT(sq[:], oT[:, ts], AF.Square, [oTb], [sqb])
                    tk = tbank()
                    MM(pst[tk][:], BONES64, sq[:], [sqb, cbfb], [psb[tk]])
                    rs, rsb = rsr.next()
                    ACT(rs[:], pst[tk][:], AF.Sqrt, [psb[tk], cstb], [rsb], bias=C_EPS6, scale=1.0)
                    RECIP(rs[:], rs[:], [rsb], [rsb])
                    yt, yb = yr.next()
                    STT(yt[:], oT[:, ts], V(l, V_SBG + hp), rs[:], ALU.mult, ALU.mult, [oTb, rsb, vecb], [yb])
                    P.dma('sp', mixT_s[1024 + hp * 128:1024 + (hp + 1) * 128, ts], yt[:], [yb], [DB('mixT', 8 + hp)], yb)

        stages = [s0, s1, s2, s3, s4, s5, s6, s7]
        units = [dict(hp=hp, tb=tb, j=j) for hp in range(8) for tb in range(NQB) for j in range(2)]
        NS = len(stages)
        for i in range(len(units) + NS - 1):
            for k in reversed(range(NS)):
                ui = i - k
                if 0 <= ui < len(units):
                    stages[k](units[ui])

    def phase_R(st, l, s):
        LW = sb("R_lw", [64, T], BF16, st)
        LA = sb("R_la", [64, T], BF16, st)
        LG = sb("R_lg", [128, T], BF16, st)
        LG2 = sb("R_lg2", [32, T], BF16, st)
        LV = sb("R_lv", [32, T], BF16, st)
        lob = Buf("R_lo")
        wup = sb("R_wup", [64, DR], BF16, st)
        aup = sb("R_aup", [64, DR], BF16, st)
        gup = sb("R_gup", [128, DR], BF16, st)
        gup2 = sb("R_gup2", [32, DR], BF16, st)
        vup = sb("R_vup", [32, DR], BF16, st)
        lwb = Buf("R_wup")
        aub = Buf("R_aup")
        gub = Buf("R_gup")
        gub2 = Buf("R_gup2")
        vub = Buf("R_vup")
        P.dma('pool', wup[:], w_up[l], [], [lwb], lwb)
        P.dma('pool', aup[:], a_up[l], [], [aub], aub)
        P.dma('pool', gup[:], g_up[l][0:128, :], [], [gub], gub)
        P.dma('pool', gup2[:], g_up[l][128:160, :], [], [gub2], gub2)
        if l > 0:
            P.dma('pool', vup[:], v_up[l - 1], [], [vub], vub)
        ldr = Ring(st, nc, "R_ld", [128, 512], F32, 4)
        for tt in range(NT):
            ts = slice(tt * 512, (tt + 1) * 512)
            t1, b1 = ldr.next()
            P.dma('sp', t1[0:64, :], xsT_s[3072:3136, ts], [DB('xsT', 24)], [b1], b1)
            ACT(LW[:, ts], t1[0:64, :], AF.Tanh, [b1], [lob])
            t2, b2 = ldr.next()
            P.dma('sp', t2[0:64, :], xsT_s[3136:3200, ts], [DB('xsT', 24)], [b2], b2)
            ACT(LA[:, ts], t2[0:64, :], AF.Copy, [b2], [lob])
            t3, b3 = ldr.next()
            P.dma('sp', t3[:], xsT_s[3200:3328, ts], [DB('xsT', 25)], [b3], b3)
            ACT(LG[:, ts], t3[:], AF.Sigmoid, [b3], [lob])
            t4, b4 = ldr.next()
            P.dma('sp', t4[0:32, :], xsT_s[3328:3360, ts], [DB('xsT', 26)], [b4], b4)
            ACT(LG2[:, ts], t4[0:32, :], AF.Sigmoid, [b4], [lob])
            if l > 0:
                t5, b5 = ldr.next()
                P.dma('sp', t5[0:32, :], xsT_s[P_R0:P_R0 + 32, ts], [DB('xsT', 26)], [b5], b5)
                ACT(LV[:, ts], t5[0:32, :], AF.Copy, [b5], [lob])

        NG = 4
        BK = [sb("R_bk%d" % h, [128, 8, 256], BF16, st) for h in range(NG)]
        AR = [sb("R_ar%d" % h, [128, 8, 256], BF16, st) for h in range(NG)]
        VT = [sb("R_vt%d" % h, [128, 8, 128], BF16, st) for h in range(NG)]
        WC = [sb("R_wc%d" % h, [128, 8], F32, st) for h in range(NG)]
        BON = [sb("R_bon%d" % h, [128, 512], F32, st) for h in range(NG)]
        YT = [sb("R_yt%d" % h, [128, 512], F32, st) for h in range(NG)]
        SF = [sb("R_sf%d" % h, [128, 128], F32, st) for h in range(8)]
        SB_ = [sb("R_sb%d" % h, [128, 128], BF16, st) for h in range(8)]
        gb_ = [{k: Buf("R_%s%d" % (k, h)) for k in ('bk', 'ar', 'vt', 'wc', 'bon', 'yt')} for h in range(NG)]
        sfb = [Buf("R_sf%d" % h) for h in range(8)]
        sbb = [Buf("R_sb%d" % h) for h in range(8)]
        for h in range(NG):
            MEMSET('pool', BK[h][:], 0.0, [gb_[h]['bk']])
            MEMSET('pool', AR[h][:], 0.0, [gb_[h]['ar']])
            MEMSET('pool', VT[h][:], 0.0, [gb_[h]['vt']])
        for h in range(8):
            MEMSET('pool', SF[h][:], 0.0, [sfb[h]])
            MEMSET('pool', SB_[h][:], 0.0, [sbb[h]])
        f32r = Ring(st, nc, "R_f", [128, 512], F32, 14)
        b16r = Ring(st, nc, "R_h", [128, 512], BF16, 4)
        mr = Ring(st, nc, "R_m", [128, 512], BF16, 3 * NG)
        pr = Ring(st, nc, "R_p", [128, 256], BF16, 3 * NG)
        qr = Ring(st, nc, "R_q", [128, 128], BF16, 4 * NG)
        tkr = Ring(st, nc, "R_tk", [128, 384], BF16, 3 * NG)
        xr_ = Ring(st, nc, "R_x", [128, 128], BF16, 2 * NG)
        ur_ = Ring(st, nc, "R_u", [128, 128], BF16, 2 * NG)
        s0r = Ring(st, nc, "R_s0", [128, 128], F32, 2 * NG)
        yo = Ring(st, nc, "R_yo", [128, 512], BF16, 3)
        pctr = [0]

        def psn():
            k = pctr[0] % 8
            pctr[0] += 1
            return pst[k], psb[k]

        ectr = [0]

        def cp(out, in_, r, w):
            ectr[0] += 1
            CPY('act' if ectr[0] % 2 == 0 else 'dve', out, in_, r, w)

        for tt in range(NT):
            ts = slice(tt * 512, (tt + 1) * 512)
            for grp in range(8 // NG):
                for g in range(NG):
                    h = grp * NG + g
                    gb = gb_[g]
                    cs = slice(h * 128, (h + 1) * 128)
                    r_, rb = f32r.next()
                    k_, kb = f32r.next()
                    v_, vb = f32r.next()
                    P.dma('sp', r_[:], xsT_s[h * 128:(h + 1) * 128, ts], [DB('xsT', h)], [rb], rb)
                    P.dma('act', k_[:], xsT_s[1024 + h * 128:1024 + (h + 1) * 128, ts], [DB('xsT', 8 + h)], [kb], kb)
                    P.dma('sp', v_[:], xsT_s[2048 + h * 128:2048 + (h + 1) * 128, ts], [DB('xsT', 16 + h)], [vb], vb)
                    ps, pb = psn()
                    MM(ps[:], wup[:, cs], LW[:, ts], [lwb, lob], [pb])
                    sg, sgb = f32r.next()
                    ACT(sg[:], ps[:], AF.Sigmoid, [pb, vecb], [sgb], bias=V(l, V_W0 + h), scale=1.0)
                    cum, cumb = f32r.next()
                    SCAN(cum[:], RESET, sg[:], 0.0, [sgb, cfb], [cumb])
                    Wt, Wtb = f32r.next()
                    iW, iWb = f32r.next()
                    Wex, Wexb = f32r.next()
                    ACT(Wt[:], cum[:], AF.Exp, [cumb], [Wtb], scale=-LAM)
                    ACT(iW[:], cum[:], AF.Exp, [cumb], [iWb], scale=LAM)
                    TT('pool', sg[:], cum[:], sg[:], ALU.subtract, [cumb, sgb], [sgb])
                    ACT(Wex[:], sg[:], AF.Exp, [sgb], [Wexb], scale=-LAM)
                    CPY('pool', WC[g][:], c3(Wt[:])[:, :, 63], [Wtb], [gb['wc']])
                    ps, pb = psn()
                    MM(ps[:], aup[:, cs], LA[:, ts], [aub, lob], [pb])
                    a_, ab = f32r.next()
                    ACT(a_[:], ps[:], AF.Sigmoid, [pb, vecb], [ab], bias=V(l, V_A0 + h), scale=1.0)
                    if l == 0:
                        P.dma('sp', vfT_s[s][h * 128:(h + 1) * 128, ts], v_[:], [vb], [DB('vf', s, h)], vb)
                    else:
                        ps, pb = psn()
                        MM(ps[:], vup[:, cs], LV[:, ts], [vub, lob], [pb])
                        sv, svb = f32r.next()
                        ACT(sv[:], ps[:], AF.Sigmoid, [pb, vecb], [svb], bias=V(l, V_V0 + h), scale=1.0)
                        vf, vfb = f32r.next()
                        P.dma('act', vf[:], vfT_s[s][h * 128:(h + 1) * 128, ts], [DB('vf', s, h)], [vfb], vfb)
                        TT('pool', vf[:], vf[:], v_[:], ALU.subtract, [vfb, vb], [vfb])
                        TT('pool', vf[:], vf[:], sv[:], ALU.mult, [vfb, svb], [vfb])
                        TT('pool', v_[:], v_[:], vf[:], ALU.add, [vfb, vb], [vb])
                    kk, kkb = f32r.next()
                    ACT(kk[:], k_[:], AF.Copy, [kb, vecb], [kkb], scale=V(l, V_KK + h))
                    sq, sqb = b16r.next()
                    ACT(sq[:], kk[:], AF.Square, [kkb], [sqb])
                    ps, pb = psn()
                    MM(ps[:], BONES, sq[:], [sqb, cbfb], [pb])
                    rs, rsb = f32r.next()
                    TS('dve', rs[:], ps[:], 1e-24, None, ALU.max, None, [pb], [rsb])
                    ACT(rs[:], rs[:], AF.Sqrt, [rsb], [rsb])
                    RECIP(rs[:], rs[:], [rsb], [rsb])
                    TT('dve', kk[:], kk[:], rs[:], ALU.mult, [kkb, rsb], [kkb])
                    km, kmb = f32r.next()
                    TS('dve', km[:], a_[:], -1.0, V(l, V_KA + h), ALU.add, ALU.mult, [ab, vecb], [kmb])
                    STT(km[:], km[:], 1.0, k_[:], ALU.add, ALU.mult, [kmb, kb], [kmb])
                    rk, rkb = b16r.next()
                    STT(rk[:], r_[:], V(l, V_RK + h), km[:], ALU.mult, ALU.mult, [rb, kmb, vecb], [rkb])
                    ps, pb = psn()
                    MM(ps[:], BONES, rk[:], [rkb, cbfb], [pb])
                    TT('dve', BON[g][:], ps[:], v_[:], ALU.mult, [pb, vb], [gb['bon']])
                    TT('pool', a_[:], a_[:], kk[:], ALU.mult, [ab, kkb], [ab])
                    for j in range(2):
                        pp = slice(64 * j, 64 * j + 64)
                        co = 64 * j
                        e1 = 'dve' if j == 0 else 'pool'
                        STT(AR[g][pp, :, co:co + 64], c3(kk[pp, :]), -1.0, c3(Wex[pp, :]), ALU.mult, ALU.mult,
                            [kkb, Wexb], [gb['ar']])
                        TT(e1, AR[g][pp, :, 128 + co:128 + co + 64], c3(r_[pp, :]), c3(Wt[pp, :]), ALU.mult,
                           [rb, Wtb], [gb['ar']])
                        TT(e1, BK[g][pp, :, co:co + 64], c3(a_[pp, :]), c3(iW[pp, :]), ALU.mult, [ab, iWb], [gb['bk']])
                        TT(e1, BK[g][pp, :, 128 + co:128 + co + 64], c3(km[pp, :]), c3(iW[pp, :]), ALU.mult,
                           [kmb, iWb], [gb['bk']])
                        ACT(VT[g][pp, :, co:co + 64], c3(v_[pp, :]), AF.Copy, [vb], [gb['vt']])
                def st1(n, stt):
                    for g in range(NG):
                        gb = gb_[g]
                        d = stt[g]
                        ps1, pb1 = psn()
                        MM(ps1[:, 0:256], BK[g][:, n, 0:128], AR[g][:, n, :], [gb['bk'], gb['ar']], [pb1])
                        MM(ps1[:, 256:512], BK[g][:, n, 128:256], AR[g][:, n, :], [gb['bk'], gb['ar']], [pb1])
                        M_, Mb = mr.next()
                        TT('dve', M_[:], ps1[:], MSK4, ALU.mult, [pb1, cfb], [Mb])
                        ps2, pb2 = psn()
                        MM(ps2[:, 0:128], AR[g][:, n, 0:128], BK[g][:, n, 0:128], [gb['bk'], gb['ar']], [pb2])
                        PP, PPb = pr.next()
                        TT('dve', PP[:, 128:256], ps2[:, 0:128], MSKT, ALU.mult, [pb2, cfb], [PPb])
                        CPY('pool', PP[:, 0:128], M_[:, 0:128], [Mb], [PPb])
                        Q_, Qb = qr.next()
                        TT('pool', Q_[:], M_[:, 0:128], IDENT, ALU.add, [Mb, cbfb], [Qb])
                        ps3, pb3 = psn()
                        pT = ps3[:].bitcast(BF16)
                        TR(pT[:, 0:128], BK[g][:, n, 0:128], [gb['bk']], [pb3])
                        TR(pT[:, 128:256], BK[g][:, n, 128:256], [gb['bk']], [pb3])
                        TR(pT[:, 256:384], VT[g][:, n, :], [gb['vt']], [pb3])
                        TK, TKb = tkr.next()
                        ACT(TK[:], pT[:, 0:384], AF.Copy, [pb3], [TKb])
                        d.update(M=M_, Mb=Mb, PP=PP, PPb=PPb, Q=Q_, Qb=Qb, TK=TK, TKb=TKb)

                def level(lev, stt):
                    for g in range(NG):
                        d = stt[g]
                        PP, PPb, Q_, Qb = d['PP'], d['PPb'], d['Q'], d['Qb']
                        ps, pb = psn()
                        if lev == 0:
                            MM(ps[:, 0:128], PP[:, 128:256], PP[:, 0:128], [PPb], [pb])
                            MM(ps[:, 128:256], PP[:, 0:128], PP[:, 128:256], [PPb], [pb])
                            PN, PNb = pr.next()
                            cp(PN[:], ps[:, 0:256], [pb], [PNb])
                            d['PP'], d['PPb'] = PN, PNb
                        elif lev < 5:
                            MM(ps[:, 256:384], PP[:, 128:256], Q_[:], [PPb, Qb], [pb])
                            MM(ps[:, 0:128], PP[:, 128:256], PP[:, 0:128], [PPb], [pb])
                            MM(ps[:, 128:256], PP[:, 0:128], PP[:, 128:256], [PPb], [pb])
                            QN, QNb = qr.next()
                            TT('dve', QN[:], ps[:, 256:384], Q_[:], ALU.add, [pb, Qb], [QNb])
                            PN, PNb = pr.next()
                            ACT(PN[:], ps[:, 0:256], AF.Copy, [pb], [PNb])
                            d['PP'], d['PPb'], d['Q'], d['Qb'] = PN, PNb, QN, QNb
                        else:
                            MM(ps[:, 0:128], PP[:, 128:256], Q_[:], [PPb, Qb], [pb])
                            QN, QNb = qr.next()
                            TT('dve', QN[:], ps[:, 0:128], Q_[:], ALU.add, [pb, Qb], [QNb])
                            d['Q'], d['Qb'] = QN, QNb

                def sq_X(n, stt):
                    for g in range(NG):
                        h = grp * NG + g
                        gb = gb_[g]
                        d = stt[g]
                        ps, pb = psn()
                        MM(ps[:, 0:128], AR[g][:, n, 0:128], SB_[h][:], [gb['ar'], sbb[h]], [pb], start=True, stop=False)
                        MM(ps[:, 0:128], d['M'][:, 256:384], d['TK'][:, 256:384], [d['Mb'], d['TKb']], [pb],
                           start=False, stop=True)
                        X_, Xb = xr_.next()
                        cp(X_[:], ps[:, 0:128], [pb], [Xb])
                        d['X'], d['Xb'] = X_, Xb

                def sq_U(n, stt):
                    for g in range(NG):
                        d = stt[g]
                        ps, pb = psn()
                        MM(ps[:, 0:128], d['Q'][:], d['X'][:], [d['Qb'], d['Xb']], [pb])
                        U_, Ub = ur_.next()
                        cp(U_[:], ps[:, 0:128], [pb], [Ub])
                        d['U'], d['Ub'] = U_, Ub

                def sq_Y(n, stt):
                    for g in range(NG):
                        h = grp * NG + g
                        gb = gb_[g]
                        d = stt[g]
                        ps, pb = psn()
                        MM(ps[:, 0:128], SB_[h][:], AR[g][:, n, 128:256], [gb['ar'], sbb[h]], [pb], start=True, stop=False)
                        MM(ps[:, 0:128], d['U'][:], d['M'][:, 128:256], [d['Ub'], d['Mb']], [pb], start=False, stop=False)
                        MM(ps[:, 0:128], d['TK'][:, 256:384], d['M'][:, 384:512], [d['TKb'], d['Mb']], [pb],
                           start=False, stop=True)
                        ACT(YT[g][0:64, n * 64:(n + 1) * 64], ps[0:64, 0:64], AF.Copy, [pb], [gb['yt']])
                        CPY('dve', YT[g][64:128, n * 64:(n + 1) * 64], ps[64:128, 64:128], [pb], [gb['yt']])

                def sq_S(n, stt):
                    for g in range(NG):
                        h = grp * NG + g
                        gb = gb_[g]
                        d = stt[g]
                        ps, pb = psn()
                        MM(ps[:, 0:128], d['TK'][:, 0:128], d['U'][:], [d['TKb'], d['Ub']], [pb], start=True, stop=False)
                        MM(ps[:, 0:128], d['TK'][:, 128:256], d['TK'][:, 256:384], [d['TKb']], [pb], start=False, stop=True)
                        S0, S0b = s0r.next()
                        TS('dve', S0[:], SF[h][:], WC[g][:, n:n + 1], None, ALU.mult, None, [sfb[h], gb['wc']], [S0b])
                        STT(SF[h][:], ps[:, 0:128], WC[g][:, n:n + 1], S0[:], ALU.mult, ALU.add,
                            [pb, S0b, gb['wc']], [sfb[h]])
                        ACT(SB_[h][:], SF[h][:], AF.Copy, [sfb[h]], [sbb[h]])

                NCK = 8 if rstop >= 2 else 0
                chs = [[dict() for _ in range(NG)] for _ in range(NCK + 1)]
                if NCK:
                    st1(0, chs[0])
                    for lev in range(6):
                        level(lev, chs[0])
                for n in range(NCK):
                    nx = n + 1 < NCK
                    if nx:
                        st1(n + 1, chs[n + 1])
                    sq_X(n, chs[n])
                    if nx:
                        level(0, chs[n + 1])
                        level(1, chs[n + 1])
                    sq_U(n, chs[n])
                    if nx:
                        level(2, chs[n + 1])
                        level(3, chs[n + 1])
                    sq_Y(n, chs[n])
                    sq_S(n, chs[n])
                    if nx:
                        level(4, chs[n + 1])
                        level(5, chs[n + 1])
                for g in range(NG if rstop >= 5 else 0):
                    h = grp * NG + g
                    gb = gb_[g]
                    cs = slice(h * 128, (h + 1) * 128)
                    yb16, yb16b = b16r.next()
                    ACT(yb16[:], YT[g][:], AF.Copy, [gb['yt']], [yb16b])
                    ps, pb = psn()
                    MM(ps[:], BONES64, yb16[:], [yb16b, cbfb], [pb])
                    dd, ddb = f32r.next()
                    TT('dve', dd[:], YT[g][:], ps[:], ALU.subtract, [gb['yt'], pb], [ddb])
                    sq, sqb = b16r.next()
                    ACT(sq[:], dd[:], AF.Square, [ddb], [sqb])
                    ps2, pb2 = psn()
                    MM(ps2[:], BONES64, sq[:], [sqb, cbfb], [pb2])
                    rs, rsb = f32r.next()
                    ACT(rs[:], ps2[:], AF.Sqrt, [pb2, cstb], [rsb], bias=C_EPSLN, scale=1.0)
                    RECIP(rs[:], rs[:], [rsb], [rsb])
                    TT('dve', dd[:], dd[:], rs[:], ALU.mult, [ddb, rsb], [ddb])
                    TS('dve', dd[:], dd[:], V(l, V_LNW + h), V(l, V_LNB + h), ALU.mult, ALU.add, [ddb, vecb], [ddb])
                    TT('pool', dd[:], dd[:], BON[g][:], ALU.add, [ddb, gb['bon']], [ddb])
                    ps3, pb3 = psn()
                    MM(ps3[:], gup[:, cs], LG[:, ts], [gub, lob], [pb3], start=True, stop=False)
                    MM(ps3[:], gup2[:, cs], LG2[:, ts], [gub2, lob], [pb3], start=False, stop=True)
                    yt, ytb = yo.next()
                    TT('dve', yt[:], ps3[:], dd[:], ALU.mult, [pb3, ddb], [ytb])
                    P.dma('sp', mixT_s[h * 128:(h + 1) * 128, ts], yt[:], [ytb], [DB('mixT', h)], ytb)

    def scoped(fn, *a):
        P.barrier()
        P.scope_begin()
        with ExitStack() as st:
            fn(st, *a)
        P.barrier()
        P.scope_end()

    def seg_in(st, l, s):
        hT = sb("hT", [128, 16, T], BF16, st)
        hTb = Buf("hT")
        with ExitStack() as st2:
            if l == 0:
                norm_pass(st2, 0, 0, l, V_PREMIX, xT_in[s], ('xin', s), None, None, None, hT, hTb)
            else:
                norm_pass(st2, l - 1, V_POSTMLP, l, V_PREMIX, xT_s[s], ('x', s), oT_s[s], xT_s[s], ('x', s), hT, hTb, ('oT', s))
        P.barrier()
        phase_A(st, l, s, hT, hTb)

    def seg_mid(st, l, s):
        hT = sb("hT", [128, 16, T], BF16, st)
        hTb = Buf("hT")
        load_rhsT(hT, hTb, mixT_s, 'mixT')
        phase_C(st, l, s, hT, hTb)

    def seg_ffn(st, l, s):
        hT = sb("hT", [128, 16, T], BF16, st)
        hTb = Buf("hT")
        src = xT_in[s] if l == 0 else xT_s[s]
        skey = ('xin', s) if l == 0 else ('x', s)
        with ExitStack() as st2:
            norm_pass(st2, l, V_POSTMIX, l, V_PREMLP, src, skey, oT_s[s], xT_s[s], ('x', s), hT, hTb, ('oT', s))
        P.barrier()
        phase_F1(st, l, hT, hTb)

    def seg_final(st, s):
        norm_pass(st, NL - 1, V_POSTMLP, 0, 0, xT_s[s], ('x', s), oT_s[s], outT[s], ('out', s), None, None, ('oT', s))

    ph = phases or "ARSCFGZ"
    for l in range(NL):
        for s in range(NSEQ):
            if 'A' in ph:
                scoped(seg_in, l, s)
            if 'R' in ph:
                scoped(phase_R, l, s)
            if 'S' in ph:
                scoped(phase_S, l)
            if 'C' in ph:
                scoped(seg_mid, l, s)
            if 'F' in ph:
                scoped(seg_ffn, l, s)
            if 'G' in ph:
                scoped(phase_F2, l, s)
            if l == NL - 1 and 'Z' in ph:
                scoped(seg_final, s)
    P.finish()
    P.emit(es)
    es.close()
    return nc


_CACHE = {}


def kernel(**inputs):
    B, T, _ = inputs['x'].shape
    NL = inputs['w_in'].shape[0]
    NCORES = 8
    NSEQ = B // NCORES
    key = (T, NL, NSEQ)
    if key not in _CACHE:
        _CACHE[key] = build(T, NL, NSEQ)
    nc = _CACHE[key]
    x = np.asarray(inputs['x'], np.float32)
    consts = make_consts()
    vecs = pack_vecs(inputs, NL)
    shared = {
        'consts': consts, 'vecs': vecs,
        'w_in': np.ascontiguousarray(inputs['w_in'], np.float32),
        'w_in_vres': np.ascontiguousarray(inputs['w_in_vres'], np.float32),
        'w_up': np.ascontiguousarray(inputs['w_up'], np.float32),
        'a_up': np.ascontiguousarray(inputs['a_up'], np.float32),
        'g_up': np.ascontiguousarray(inputs['g_up'], np.float32),
        'v_up': np.ascontiguousarray(inputs['v_up'], np.float32),
        'w_out': np.ascontiguousarray(inputs['w_out'], np.float32),
        'w_ff_up': np.ascontiguousarray(inputs['w_ff_up'], np.float32),
        'w_ff_down': np.ascontiguousarray(inputs['w_ff_down'], np.float32),
    }
    in_maps = []
    for c in range(NCORES):
        m = dict(shared)
        m['xT'] = np.ascontiguousarray(np.transpose(x[c * NSEQ:(c + 1) * NSEQ], (0, 2, 1)))
        in_maps.append(m)
    res = run_bass_kernel_spmd(nc, in_maps, core_ids=list(range(NCORES)))
    outs = [np.transpose(r['outT'], (0, 2, 1)) for r in res.results]
    return np.ascontiguousarray(np.concatenate(outs, axis=0)).astype(np.float32)
```

```python
import numpy as np
import os
from contextlib import ExitStack
import concourse.bass as bass
import concourse.mybir as mybir
from concourse.bass_utils import run_bass_kernel_spmd

F32 = mybir.dt.float32
BF16 = mybir.dt.bfloat16
AF = mybir.ActivationFunctionType
ALU = mybir.AluOpType

D = 2048
DR = 1024
P_R0 = 3360
P_IN = 6432
DFF = 8192
LAM = float(np.exp(-0.5))
ENG = ['pe', 'act', 'dve', 'pool', 'sp']
SAME_SYNC = ('act', 'dve', 'pool')
SAME_ALL = bool(int(os.environ.get('SAME_ALL', '0')))

C_ID, C_BONES, C_STRICT, C_INCL, C_STRICTT, C_SBMASK, C_RESET = 0, 128, 256, 384, 768, 896, 1024
NCONST = 1024 + 512
V_PREMIX, V_POSTMIX, V_PREMLP, V_POSTMLP = 0, 16, 32, 48
V_MU = 64
V_MUV = 91
V_W0, V_A0, V_KK, V_KA, V_LNW, V_LNB, V_V0, V_RK, V_SBG = 92, 100, 108, 116, 124, 132, 140, 148, 156
NV = 164


def make_consts():
    c = np.zeros((128, NCONST), np.float32)
    p = np.arange(128)
    c[:, C_ID:C_ID + 128] = np.eye(128, dtype=np.float32)
    same = (p[:, None] // 64) == (p[None, :] // 64)
    c[:, C_BONES:C_BONES + 128] = same
    i = p[:, None] % 64
    t = p[None, :] % 64
    c[:, C_STRICT:C_STRICT + 128] = same & (i < t)
    c[:, C_INCL:C_INCL + 128] = same & (i <= t)
    c[:, C_STRICT + 256:C_STRICT + 384] = same & (i < t)
    c[:, C_INCL + 256:C_INCL + 384] = same & (i <= t)
    c[:, C_STRICTT:C_STRICTT + 128] = same & (i > t)
    c[:, C_SBMASK:C_SBMASK + 128] = (p[None, :] < p[:, None])
    c[:, C_RESET:C_RESET + 512] = (np.arange(512)[None, :] % 64 != 0)
    return c


def pack_vecs(inp, nl):
    v = np.zeros((128, nl * NV), np.float32)

    def put(l, col, arr, n):
        a = np.zeros(n * 128, np.float32)
        arr = np.asarray(arr, np.float32).reshape(-1)
        a[:arr.size] = arr
        v[:, l * NV + col:l * NV + col + n] = a.reshape(n, 128).T

    for l in range(nl):
        put(l, V_PREMIX, inp['pre_mix_g'][l], 16)
        put(l, V_POSTMIX, inp['post_mix_g'][l], 16)
        put(l, V_PREMLP, inp['pre_mlp_g'][l], 16)
        put(l, V_POSTMLP, inp['post_mlp_g'][l], 16)
        put(l, V_MU, inp['mu'][l], 27)
        put(l, V_W0, inp['w0'][l], 8)
        put(l, V_A0, inp['a0'][l], 8)
        put(l, V_KK, inp['k_k'][l], 8)
        put(l, V_KA, inp['k_a'][l], 8)
        put(l, V_LNW, inp['lnx_w'][l], 8)
        put(l, V_LNB, inp['lnx_b'][l], 8)
        put(l, V_RK, inp['r_k'][l], 8)
        put(l, V_SBG, inp['sb_out_g'][l], 8)
        if l > 0:
            put(l, V_MUV, inp['mu_vres'][l - 1], 1)
            put(l, V_V0, inp['v0'][l - 1], 8)
    return v


class Buf:
    __slots__ = ('name', 'w', 'r', 'chan', 'excl')

    def __init__(self, name, excl=False):
        self.name = name
        self.w = None
        self.r = {}
        self.chan = None
        self.excl = excl


class Prog:
    def __init__(self, nc):
        self.nc = nc
        self.ins = {e: [] for e in ENG}
        self.chan_count = []
        self.pending = {e: None for e in ENG}
        self.last_tok = {}
        self.free_ch = []
        self.scope_ch = None

    def _chan(self, buf):
        if buf.chan is None:
            if self.free_ch:
                buf.chan = self.free_ch.pop()
            else:
                buf.chan = len(self.chan_count)
                self.chan_count.append(0)
            if self.scope_ch is not None:
                self.scope_ch.append(buf.chan)
        return buf.chan

    def scope_begin(self):
        self.scope_ch = []

    def scope_end(self):
        self.free_ch.extend(self.scope_ch)
        self.scope_ch = None

    def op(self, eng, meth, reads=(), writes=(), chan_buf=None, same=False, **kw):
        lst = self.ins[eng]
        deps = set()
        if any(b.excl for b in reads):
            writes = list(writes) + [b for b in reads if b.excl and b not in writes]
            reads = [b for b in reads if not b.excl]
        for b in reads:
            if b.w is not None:
                deps.add(b.w)
        ch0 = self._chan(chan_buf) if chan_buf is not None else None
        for b in writes:
            if b.w is not None:
                if not (b is chan_buf and b.w[0] == 'c' and b.w[1] == ch0):
                    deps.add(b.w)
            deps.update(b.r.values())
        if self.pending[eng] is not None:
            deps |= self.pending[eng]
            self.pending[eng] = None
        if chan_buf is not None:
            ch = self._chan(chan_buf)
            self.chan_count[ch] += 1
            tok = ('c', ch, self.chan_count[ch])
            key = ('c', ch)
        else:
            ch = None
            tok = (eng, len(lst))
            key = eng
        lst.append({'fn': meth, 'kw': kw, 'deps': deps, 'chan': ch, 'signal': False, 'same': same})
        self.last_tok[key] = tok
        for b in reads:
            b.r[key] = tok
        for b in writes:
            b.w = tok
            b.r = {}
        return tok

    def dma(self, eng, out, in_, reads, writes, chan_buf):
        return self.op(eng, 'dma_start', reads, writes, chan_buf, out=out, in_=in_)

    def barrier(self):
        allt = set(self.last_tok.values())
        for e in ENG:
            self.pending[e] = set(allt) | (self.pending[e] or set())

    def finish(self):
        self.barrier()
        self.op('sp', None)

    def emit(self, es):
        nc = self.nc
        comp = ['pe', 'act', 'dve', 'pool']
        for e in ENG:
            for rec in self.ins[e]:
                for tok in rec['deps']:
                    if tok[0] == 'c':
                        continue
                    e2, i2 = tok
                    if e2 == e and not (e in SAME_SYNC and (rec['chan'] is not None or SAME_ALL or rec['same'])):
                        continue
                    self.ins[e2][i2]['signal'] = True
        for e in comp:
            cnt = 0
            for rec in self.ins[e]:
                if rec['signal']:
                    cnt += 1
                    rec['sigval'] = cnt
        esem = {e: es.enter_context(nc.semaphore('s_' + e)) for e in comp}
        csem = [es.enter_context(nc.semaphore('c_%d' % i)) for i in range(len(self.chan_count))]
        block = es.enter_context(nc.Block())
        prog = self

        def run(e, eo):
            known = {}
            for rec in prog.ins[e]:
                waits = {}
                for tok in rec['deps']:
                    if tok[0] == 'c':
                        key = ('c', tok[1])
                        val = 16 * tok[2]
                        sem = csem[tok[1]]
                    else:
                        e2, i2 = tok
                        if e2 == e and not (e in SAME_SYNC and (rec['chan'] is not None or SAME_ALL or rec['same'])):
                            continue
                        key = e2
                        val = prog.ins[e2][i2]['sigval']
                        sem = esem[e2]
                    if known.get(key, 0) >= val:
                        continue
                    if key not in waits or waits[key][0] < val:
                        waits[key] = (val, sem)
                for key, (val, sem) in waits.items():
                    known[key] = val
                    eo.wait_ge(sem, val)
                if rec['fn'] is None:
                    continue
                ins = getattr(eo, rec['fn'])(**rec['kw'])
                if rec['chan'] is not None:
                    ins.then_inc(csem[rec['chan']], 16)
                elif rec['signal']:
                    ins.then_inc(esem[e], 1)

        @block.tensor
        def _(eo):
            run('pe', eo)

        @block.scalar
        def _(eo):
            run('act', eo)

        @block.vector
        def _(eo):
            run('dve', eo)

        @block.gpsimd
        def _(eo):
            run('pool', eo)

        @block.sync
        def _(eo):
            run('sp', eo)


_UID = [0]


def _uid():
    _UID[0] += 1
    return _UID[0]


class Ring:
    def __init__(self, es, nc, name, shape, dtype, n):
        u = _uid()
        self.t = [es.enter_context(nc.sbuf_tensor('%s_%d_%d' % (name, u, i), shape, dtype)) for i in range(n)]
        self.b = [Buf('%s%d' % (name, i)) for i in range(n)]
        self.i = 0

    def next(self):
        k = self.i % len(self.t)
        self.i += 1
        return self.t[k], self.b[k]


class PsRing:
    def __init__(self, tens, bufs):
        self.t = tens
        self.b = bufs
        self.i = 0

    def next(self):
        k = self.i % len(self.t)
        self.i += 1
        return self.t[k], self.b[k]


def build(T, NL, NSEQ, debug=False, phases=None, rstop=99):
    assert T % 512 == 0
    NT = T // 512
    NQB = T // 128
    HT = min(1024, T)
    nc = bass.Bass("TRN2", target_bir_lowering=False)
    es = ExitStack()
    P = Prog(nc)

    def din(name, shape, dt=F32):
        return nc.dram_tensor(name, list(shape), dt, kind="ExternalInput").ap()

    def dscr(name, shape, dt=F32):
        kind = "ExternalOutput" if debug else "Internal"
        return nc.dram_tensor(name, list(shape), dt, kind=kind).ap()

    xT_in = din("xT", [NSEQ, D, T])
    consts_d = din("consts", [128, NCONST])
    vecs_d = din("vecs", [128, NL * NV])
    w_in = din("w_in", [NL, D, P_IN])
    w_in_vres = din("w_in_vres", [max(NL - 1, 1), D, 32])
    w_up = din("w_up", [NL, 64, DR])
    a_up = din("a_up", [NL, 64, DR])
    g_up = din("g_up", [NL, 160, DR])
    v_up = din("v_up", [max(NL - 1, 1), 32, DR])
    w_out = din("w_out", [NL, D, D])
    w_ff_up = din("w_ff_up", [NL, D, DFF])
    w_ff_down = din("w_ff_down", [NL, DFF, D])
    outT = nc.dram_tensor("outT", [NSEQ, D, T], F32, kind="ExternalOutput").ap()

    xT_s = dscr("xT_s", [NSEQ, D, T])
    oT_s = dscr("oT_s", [NSEQ, D, T])
    xsT_s = dscr("xsT_s", [3392, T])
    qT_s = dscr("qT_s", [1024, T], BF16)
    kT_s = dscr("kT_s", [1024, T], BF16)
    vtok_s = dscr("vtok_s", [T, 1024], BF16)
    mixT_s = dscr("mixT_s", [D, T], BF16)
    ffT_s = dscr("ffT_s", [DFF, T], BF16)
    vfT_s = dscr("vfT_s", [NSEQ, DR, T])

    dbufs = {}

    def DB(*key):
        if key not in dbufs:
            dbufs[key] = Buf(str(key))
        return dbufs[key]

    def sb(name, shape, dt, stack=None):
        return (stack or es).enter_context(nc.sbuf_tensor('%s_%d' % (name, _uid()), list(shape), dt))

    def ACT(out, in_, func, r, w, **kw):
        P.op('act', 'activation', r, w, out=out, in_=in_, func=func, **kw)

    def MM(out, lhsT, rhs, r, w, start=True, stop=True):
        P.op('pe', 'matmul', r, w, out=out, lhsT=lhsT, rhs=rhs, start=start, stop=stop)

    def TR(out, in_, r, w):
        P.op('pe', 'transpose', r + [cbfb], w, out=out, in_=in_, identity=IDENT)

    def TT(eng, out, in0, in1, op, r, w):
        P.op(eng, 'tensor_tensor', r, w, out=out, in0=in0, in1=in1, op=op)

    def TS(eng, out, in0, s1, s2, op0, op1, r, w):
        if s2 is None:
            P.op(eng, 'tensor_scalar', r, w, out=out, in0=in0, scalar1=s1, scalar2=None, op0=op0)
        else:
            P.op(eng, 'tensor_scalar', r, w, out=out, in0=in0, scalar1=s1, scalar2=s2, op0=op0, op1=op1)

    def STT(out, in0, scalar, in1, op0, op1, r, w):
        P.op('dve', 'scalar_tensor_tensor', r, w, out=out, in0=in0, scalar=scalar, in1=in1, op0=op0, op1=op1)

    def CPY(eng, out, in_, r, w):
        if eng == 'act':
            ACT(out, in_, AF.Copy, r, w)
        else:
            P.op(eng, 'tensor_copy', r, w, out=out, in_=in_)

    def RECIP(out, in_, r, w):
        P.op('dve', 'reciprocal', r, w, out=out, in_=in_)

    def MEMSET(eng, ap, val, w):
        P.op(eng, 'memset', [], w, ap=ap, constant=val)

    def SCAN(out, d0, d1, init, r, w):
        P.op('dve', 'tensor_tensor_scan', r, w, same=not isinstance(init, float), out=out, data0=d0, data1=d1,
             initial=init, op0=ALU.mult, op1=ALU.add)

    cf = sb("cf", [128, NCONST], F32)
    cfb = Buf("cf")
    vec = sb("vec", [128, NL * NV], F32)
    vecb = Buf("vec")
    omu = sb("omu", [128, NL * 28], F32)
    cbf = sb("cbf", [128, 4 * 128], BF16)
    cbfb = Buf("cbf")
    cst = sb("cst", [128, 8], F32)
    cstb = Buf("cst")
    ones_f = sb("ones_f", [128, 1024], F32)
    onesfb = Buf("onesf")

    P.dma('sp', cf[:], consts_d[:, :], [], [cfb], cfb)
    P.dma('sp', vec[:], vecs_d[:, :], [], [vecb], vecb)
    IDENT = cbf[:, 0:128]
    ONESB = cbf[:, 128:256]
    BONES = cbf[:, 256:384]
    BONES64 = cbf[:, 384:512]
    CPY('dve', cbf[:, 0:128], cf[:, C_ID:C_ID + 128], [cfb], [cbfb])
    MEMSET('dve', cbf[:, 128:256], 1.0, [cbfb])
    CPY('dve', cbf[:, 256:384], cf[:, C_BONES:C_BONES + 128], [cfb], [cbfb])
    TS('dve', cbf[:, 384:512], cf[:, C_BONES:C_BONES + 128], 1.0 / 64, None, ALU.mult, None, [cfb], [cbfb])
    MEMSET('dve', cst[:, 0:1], 1.0, [cstb])
    MEMSET('dve', cst[:, 1:2], 1e-6, [cstb])
    MEMSET('dve', cst[:, 2:3], 64e-5, [cstb])
    MEMSET('dve', ones_f[:], 1.0, [onesfb])
    for l in range(NL):
        TS('dve', omu[:, l * 28:l * 28 + 28], vec[:, l * NV + V_MU:l * NV + V_MU + 28], -1.0, 1.0, ALU.mult, ALU.add,
           [vecb], [vecb])
    C_ONE = cst[:, 0:1]
    C_EPS6 = cst[:, 1:2]
    C_EPSLN = cst[:, 2:3]
    MSK4 = cf[:, C_STRICT:C_STRICT + 512]
    MSKT = cf[:, C_STRICTT:C_STRICTT + 128]
    SBM = cf[:, C_SBMASK:C_SBMASK + 128]
    RESET = cf[:, C_RESET:C_RESET + 512]

    def V(l, col, n=1):
        return vec[:, l * NV + col:l * NV + col + n]

    pst = [es.enter_context(nc.psum_tensor('ps%d' % i, [128, 512], F32)) for i in range(8)]
    psb = [Buf('ps%d' % i, excl=True) for i in range(8)]

    def xview(ap2d):
        return ap2d.rearrange("(kc p) t -> p kc t", p=128)

    def c3(ap):
        return ap.rearrange("p (n c) -> p n c", c=64)

    NW = 256

    def rms_rstd(src3, srcb, sqr, rsr, psr):
        ps, pb = psr.next()
        for kc in range(16):
            sq, sqb = sqr.next()
            ACT(sq[:], src3[:, kc, :], AF.Square, [srcb], [sqb])
            MM(ps[:, 0:NW], ONESB, sq[:], [sqb, cbfb], [pb], start=(kc == 0), stop=(kc == 15))
        rs, rsb = rsr.next()
        ACT(rs[:], ps[:, 0:NW], AF.Sqrt, [pb, cstb], [rsb], bias=C_EPS6, scale=1.0 / D)
        RECIP(rs[:], rs[:], [rsb], [rsb])
        return rs, rsb

    def norm_pass(st, l_post, gpost_col, l_pre, gpre_col, src_x, src_key, o_ap, dst_x, dst_key, hT, hTb, o_key=None):
        xr = Ring(st, nc, "np_x", [128, 16, NW], F32, 2)
        orr = Ring(st, nc, "np_o", [128, 16, NW], F32, 2) if o_ap is not None else None
        sqr = Ring(st, nc, "np_sq", [128, NW], BF16, 4)
        rsr = Ring(st, nc, "np_rs", [128, NW], F32, 3)
        psr = PsRing(pst[0:2], psb[0:2])
        for t2 in range(T // NW):
            tt = (t2 * NW) // 512
            ts = slice(t2 * NW, (t2 + 1) * NW)
            xt, xb = xr.next()
            for q4 in range(2):
                P.dma('sp', xt[:, q4 * 8:(q4 + 1) * 8, :], xview(src_x)[:, q4 * 8:(q4 + 1) * 8, ts],
                      [DB(src_key, tt)], [xb], xb)
            if o_ap is not None:
                ot, ob = orr.next()
                for q4 in range(2):
                    P.dma('act', ot[:, q4 * 8:(q4 + 1) * 8, :], xview(o_ap)[:, q4 * 8:(q4 + 1) * 8, ts],
                          [DB(o_key, tt)], [ob], ob)
                rs, rsb = rms_rstd(ot, ob, sqr, rsr, psr)
                for kc in range(16):
                    STT(ot[:, kc, :], ot[:, kc, :], V(l_post, gpost_col + kc), rs[:], ALU.mult, ALU.mult,
                        [ob, rsb, vecb], [ob])
                    TT('dve', xt[:, kc, :], xt[:, kc, :], ot[:, kc, :], ALU.add, [ob, xb], [xb])
                if dst_x is not None:
                    for q4 in range(2):
                        P.dma('sp', xview(dst_x)[:, q4 * 8:(q4 + 1) * 8, ts], xt[:, q4 * 8:(q4 + 1) * 8, :],
                              [xb], [DB(dst_key, tt)], xb)
            if hT is not None:
                rs, rsb = rms_rstd(xt, xb, sqr, rsr, psr)
                for kc in range(16):
                    STT(hT[:, kc, ts], xt[:, kc, :], V(l_pre, gpre_col + kc), rs[:], ALU.mult, ALU.mult,
                        [xb, rsb, vecb], [hTb])

    def dense_fm(wr, rhsT, rhsb, groups, evac):
        psr = PsRing(pst[2:8], psb[2:8])
        for (W, f0, n, chunks) in groups:
            wt, wb = wr.next()
            Wv = W.rearrange("(kc p) f -> p kc f", p=128)
            for q4 in range(4):
                P.dma('pool', wt[:, q4 * 4:(q4 + 1) * 4, 0:n], Wv[:, q4 * 4:(q4 + 1) * 4, f0:f0 + n], [], [wb], wb)
            for (coff, fsz, info) in chunks:
                for tt in range(NT):
                    ps, pb = psr.next()
                    for kc in range(16):
                        MM(ps[0:fsz, :], wt[:, kc, coff:coff + fsz], rhsT[:, kc, tt * 512:(tt + 1) * 512],
                           [wb, rhsb], [pb], start=(kc == 0), stop=(kc == 15))
                    evac(info, fsz, tt, ps, pb)

    def phase_A(st, l, s, hT, hTb):
        wr = Ring(st, nc, "A_w", [128, 16, 512], BF16, 2)
        stA = Ring(st, nc, "A_a", [128, T], F32, 2)
        stB = Ring(st, nc, "A_b", [128, T + 1], F32, 2)
        stQ = Ring(st, nc, "A_q", [128, T], BF16, 2)
        stV = Ring(st, nc, "A_v", [128, 512], BF16, 3)
        for t_, b_ in zip(stB.t, stB.b):
            MEMSET('pool', t_[:, 0:1], 0.0, [b_])
        cur = {}
        W = w_in[l]

        def evac(info, fsz, tt, ps, pb):
            kind, idx = info
            ts = slice(tt * 512, (tt + 1) * 512)
            if kind in ('r', 'vres'):
                if tt == 0:
                    cur['A'] = stA.next()
                    cur['B'] = stB.next()
                (At, Ab), (Bt, Bb) = cur['A'], cur['B']
                if kind == 'r':
                    mu_ap = V(l, V_MU + idx)
                    omu_ap = omu[:, l * 28 + idx:l * 28 + idx + 1]
                    row0 = idx * 128
                else:
                    mu_ap = V(l, V_MUV)
                    omu_ap = omu[:, l * 28 + 27:l * 28 + 28]
                    row0 = P_R0
                ACT(At[0:fsz, ts], ps[0:fsz, :], AF.Copy, [pb, vecb], [Ab], scale=omu_ap[0:fsz, :])
                ACT(Bt[0:fsz, 1 + tt * 512:1 + (tt + 1) * 512], ps[0:fsz, :], AF.Copy, [pb, vecb], [Bb],
                    scale=mu_ap[0:fsz, :])
                if tt == NT - 1:
                    TT('dve', At[0:fsz, :], At[0:fsz, :], Bt[0:fsz, 0:T], ALU.add, [Ab, Bb], [Ab])
                    P.dma('sp', xsT_s[row0:row0 + fsz, :], At[0:fsz, :], [Ab], [DB('xsT', row0 // 128)], Ab)
            else:
                if tt == 0:
                    cur['Q'] = stQ.next()
                Qt, Qb = cur['Q']
                sc = 0.125 if kind == 'q' else 1.0
                ACT(Qt[:, ts], ps[:], AF.Copy, [pb], [Qb], scale=sc)
                if tt == NT - 1:
                    dst = qT_s if kind == 'q' else kT_s
                    P.dma('sp', dst[idx * 128:(idx + 1) * 128, :], Qt[:], [Qb], [DB(kind + 'T', idx)], Qb)

        groups = []
        for g in range(6):
            groups.append((W, g * 512, 512, [(c * 128, 128, ('r', g * 4 + c)) for c in range(4)]))
        groups.append((W, 3072, 288, [(0, 128, ('r', 24)), (128, 128, ('r', 25)), (256, 32, ('r', 26))]))
        for g in range(2):
            groups.append((W, P_R0 + g * 512, 512, [(c * 128, 128, ('q', g * 4 + c)) for c in range(4)]))
        for g in range(2):
            groups.append((W, P_R0 + 1024 + g * 512, 512, [(c * 128, 128, ('k', g * 4 + c)) for c in range(4)]))
        if l > 0:
            groups.append((w_in_vres[l - 1], 0, 32, [(0, 32, ('vres', 0))]))
        dense_fm(wr, hT, hTb, groups, evac)

        psr = PsRing(pst[2:8], psb[2:8])
        Wv = W.rearrange("(kc p) f -> p kc f", p=128)
        for g in range(2):
            wt, wb = wr.next()
            f0 = P_R0 + 2048 + g * 512
            for q4 in range(4):
                P.dma('pool', wt[:, q4 * 4:(q4 + 1) * 4, :], Wv[:, q4 * 4:(q4 + 1) * 4, f0:f0 + 512], [], [wb], wb)
            for tb in range(NQB):
                ps, pb = psr.next()
                for kc in range(16):
                    MM(ps[:], hT[:, kc, tb * 128:(tb + 1) * 128], wt[:, kc, :], [wb, hTb], [pb],
                       start=(kc == 0), stop=(kc == 15))
                vt, vb = stV.next()
                ACT(vt[:], ps[:], AF.Copy, [pb], [vb])
                P.dma('sp', vtok_s[tb * 128:(tb + 1) * 128, g * 512:(g + 1) * 512], vt[:], [vb],
                      [DB('vtok', tb // 4)], vb)

    def load_rhsT(hT, hTb, src, key):
        for q4 in range(4):
            for tt in range(NT):
                P.dma('sp', hT[:, q4 * 4:(q4 + 1) * 4, tt * 512:(tt + 1) * 512],
                      xview(src)[:, q4 * 4:(q4 + 1) * 4, tt * 512:(tt + 1) * 512],
                      [DB(key, kc) for kc in range(q4 * 4, q4 * 4 + 4)], [hTb], hTb)

    def phase_C(st, l, s, hT, hTb):
        wr = Ring(st, nc, "C_w", [128, 16, 512], BF16, 2)
        stO = Ring(st, nc, "C_o", [128, T], F32, 2)
        cur = {}

        def evac(info, fsz, tt, ps, pb):
            if tt == 0:
                cur['O'] = stO.next()
            Ot, Ob = cur['O']
            ACT(Ot[:, tt * 512:(tt + 1) * 512], ps[:], AF.Copy, [pb], [Ob])
            if tt == NT - 1:
                P.dma('sp', oT_s[s][info * 128:(info + 1) * 128, :], Ot[:], [Ob], [DB(('oT', s), t_) for t_ in range(NT)], Ob)

        groups = [(w_out[l], g * 512, 512, [(c * 128, 128, g * 4 + c) for c in range(4)]) for g in range(4)]
        dense_fm(wr, hT, hTb, groups, evac)

    def phase_F1(st, l, hT, hTb):
        wr = Ring(st, nc, "F1_w", [128, 16, 512], BF16, 2)
        stR = Ring(st, nc, "F_r", [128, 512], F32, 3)
        stF = Ring(st, nc, "F_f", [128, T], BF16, 2)
        cur = {}

        def evac(info, fsz, tt, ps, pb):
            if tt == 0:
                cur['F'] = stF.next()
            Ft, Fb = cur['F']
            rt, rb = stR.next()
            ACT(rt[:], ps[:], AF.Relu, [pb], [rb])
            if tt % 2 == 0:
                TT('dve', Ft[:, tt * 512:(tt + 1) * 512], rt[:], rt[:], ALU.mult, [rb], [Fb])
            else:
                ACT(Ft[:, tt * 512:(tt + 1) * 512], rt[:], AF.Square, [rb], [Fb])
            if tt == NT - 1:
                P.dma('sp', ffT_s[info * 128:(info + 1) * 128, :], Ft[:], [Fb], [DB('ffT', info)], Fb)

        groups = [(w_ff_up[l], g * 512, 512, [(c * 128, 128, g * 4 + c) for c in range(4)]) for g in range(16)]
        dense_fm(wr, hT, hTb, groups, evac)

    def phase_F2(st, l, s):
        ffh = sb("F2_ff", [128, 64, HT], BF16, st)
        ffq = [Buf("F2_ff%d" % q) for q in range(4)]
        wr = Ring(st, nc, "F2_w", [128, 64, 128], BF16, 2)
        stO = Ring(st, nc, "F2_o", [128, HT], F32, 2)
        NJ = HT // 512
        bi = 0
        Wv = w_ff_down[l].rearrange("(kc p) f -> p kc f", p=128)
        fv = ffT_s.rearrange("(kc p) t -> p kc t", p=128)
        for half in range(T // HT):
            hs = slice(half * HT, (half + 1) * HT)
            for q in range(16):
                P.dma('sp' if q % 2 == 0 else 'act', ffh[:, q * 4:(q + 1) * 4, :], fv[:, q * 4:(q + 1) * 4, hs],
                      [DB('ffT', kc) for kc in range(q * 4, q * 4 + 4)], [ffq[q // 4]], ffq[q // 4])
            for fc in range(16):
                wt, wb = wr.next()
                for q in range(4):
                    P.dma('pool', wt[:, q * 16:(q + 1) * 16, :], Wv[:, q * 16:(q + 1) * 16, fc * 128:(fc + 1) * 128],
                          [], [wb], wb)
                pss = []
                for j in range(NJ):
                    pss.append((pst[bi % 8], psb[bi % 8]))
                    bi += 1
                for kc in range(64):
                    for j in range(NJ):
                        ps, pb = pss[j]
                        MM(ps[:], wt[:, kc, :], ffh[:, kc, j * 512:(j + 1) * 512], [wb, ffq[kc // 16]], [pb],
                           start=(kc == 0), stop=(kc == 63))
                Ot, Ob = stO.next()
                for j in range(NJ):
                    ps, pb = pss[j]
                    ACT(Ot[:, j * 512:(j + 1) * 512], ps[:], AF.Copy, [pb], [Ob])
                P.dma('sp', oT_s[s][fc * 128:(fc + 1) * 128, hs], Ot[:], [Ob],
                      [DB(('oT', s), t_) for t_ in range(half * NJ, (half + 1) * NJ)], Ob)

    def phase_S(st, l):
        q2r = Ring(st, nc, "S_q", [128, T], BF16, 2)
        k2r = Ring(st, nc, "S_k", [128, T], BF16, 2)
        v2r = Ring(st, nc, "S_v", [128, NQB, 128], BF16, 2)
        er = Ring(st, nc, "S_e", [128, T], F32, 5)
        spr = Ring(st, nc, "S_sp", [128, 1024], F32, 6)
        psr_ = Ring(st, nc, "S_ps", [128, T + 1], F32, 4)
        attr = Ring(st, nc, "S_att", [128, T], BF16, 3)
        attTr = Ring(st, nc, "S_attT", [128, NQB, 128], BF16, 3)
        ntr = Ring(st, nc, "S_nt", [128, 1], F32, 6)
        o2r = Ring(st, nc, "S_o2", [128, 128], BF16, 2)
        oTr = Ring(st, nc, "S_oT", [128, T], BF16, 2)
        sqr = Ring(st, nc, "S_sq", [128, 512], BF16, 2)
        rsr = Ring(st, nc, "S_rs", [128, 512], F32, 2)
        yr = Ring(st, nc, "S_y", [128, 512], BF16, 2)
        vv = vtok_s.rearrange("(sb s) c -> s sb c", s=128)
        cnt = {'z': 0, 't': 0, 'e': 0}
        hpd = {}

        def zpair():
            k = (cnt['z'] % 2) * 2
            cnt['z'] += 1
            return k

        def tbank():
            k = 4 + (cnt['t'] % 2)
            cnt['t'] += 1
            return k

        def segs(nk):
            return [(c0, min(1024, nk - c0)) for c0 in range(0, nk, 1024)]

        def s0(u):
            hp, tb, j = u['hp'], u['tb'], u['j']
            if tb == 0 and j == 0:
                q2, q2b = q2r.next()
                k2, k2b = k2r.next()
                v2, v2b = v2r.next()
                P.dma('sp', q2[:], qT_s[hp * 128:(hp + 1) * 128, :], [DB('qT', hp)], [q2b], q2b)
                P.dma('sp', k2[:], kT_s[hp * 128:(hp + 1) * 128, :], [DB('kT', hp)], [k2b], k2b)
                P.dma('act', v2[:], vv[:, :, hp * 128:(hp + 1) * 128], [DB('vtok', i) for i in range((NQB + 3) // 4)],
                      [v2b], v2b)
                oT, oTb = oTr.next()
                hpd[hp] = dict(q2=q2, q2b=q2b, k2=k2, k2b=k2b, v2=v2, v2b=v2b, oT=oT, oTb=oTb)
            h = hpd[hp]
            nk = (tb + 1) * 128
            u['nk'] = nk
            pbs = slice(64 * j, 64 * j + 64)
            u['z'] = []
            for (c0, n) in segs(nk):
                zb = zpair()
                for hb in range((n + 511) // 512):
                    nn = min(512, n - hb * 512)
                    MM(pst[zb + hb][:, 0:nn], h['q2'][pbs, tb * 128:(tb + 1) * 128],
                       h['k2'][pbs, c0 + hb * 512:c0 + hb * 512 + nn], [h['q2b'], h['k2b']], [psb[zb + hb]])
                u['z'].append(zb)

        def s1(u):
            nk = u['nk']
            E, Eb = er.next()
            u['E'], u['Eb'] = E, Eb
            u['SP'] = []
            for si, (c0, n) in enumerate(segs(nk)):
                zb = u['z'][si]
                for hb in range((n + 511) // 512):
                    nn = min(512, n - hb * 512)
                    ACT(E[:, c0 + hb * 512:c0 + hb * 512 + nn], pst[zb + hb][:, 0:nn], AF.Exp, [psb[zb + hb]], [Eb])
                SP, SPb = spr.next()
                ACT(SP[:, 0:n], E[:, c0:c0 + n], AF.Ln, [Eb, cstb], [SPb], bias=C_ONE, scale=1.0)
                u['SP'].append((SP, SPb))

        def s2(u):
            nk = u['nk']
            PS, PSb = psr_.next()
            u['PS'], u['PSb'] = PS, PSb
            MEMSET('pool', PS[:, 0:1], 0.0, [PSb])
            for si, (c0, n) in enumerate(segs(nk)):
                SP, SPb = u['SP'][si]
                if c0 + n == nk:
                    TT('pool', SP[:, n - 128:n], SP[:, n - 128:n], SBM, ALU.mult, [SPb, cfb], [SPb])
                SCAN(PS[:, 1 + c0:1 + c0 + n], ones_f[:, 0:n], SP[:, 0:n],
                     (PS[:, c0:c0 + 1] if c0 > 0 else 0.0), [SPb, onesfb, PSb], [PSb])

        def s3(u):
            nk = u['nk']
            PS, PSb = u['PS'], u['PSb']
            NTt, NTb = ntr.next()
            TS('pool', NTt[:], PS[:, nk:nk + 1], -1.0, None, ALU.mult, None, [PSb], [NTb])
            for (c0, n) in segs(nk):
                ACT(PS[:, c0:c0 + n], PS[:, c0:c0 + n], AF.Exp, [PSb, NTb], [PSb], bias=NTt[:], scale=1.0)

        def s4(u):
            nk = u['nk']
            ATT, ATTb = attr.next()
            u['ATT'], u['ATTb'] = ATT, ATTb
            cnt['e'] += 1
            nA = (nk // 256) * 128
            if nA > 0:
                TT('pool', ATT[:, 0:nA], u['E'][:, 0:nA], u['PS'][:, 0:nA], ALU.mult, [u['Eb'], u['PSb']], [ATTb])
            TT('dve', ATT[:, nA:nk], u['E'][:, nA:nk], u['PS'][:, nA:nk], ALU.mult, [u['Eb'], u['PSb']], [ATTb])
            TT('pool', ATT[:, nk - 128:nk], ATT[:, nk - 128:nk], SBM, ALU.mult, [ATTb, cfb], [ATTb])

        def s5(u):
            tb = u['tb']
            ATT, ATTb = u['ATT'], u['ATTb']
            ATTT, ATTTb = attTr.next()
            u['ATTT'], u['ATTTb'] = ATTT, ATTTb
            for g8 in range((tb + 8) // 8):
                nb = min(8, tb + 1 - g8 * 8)
                tk = tbank()
                pT = pst[tk][:].bitcast(BF16)
                for s8 in range(nb):
                    sbk = g8 * 8 + s8
                    TR(pT[:, s8 * 128:(s8 + 1) * 128], ATT[:, sbk * 128:(sbk + 1) * 128], [ATTb], [psb[tk]])
                CPY('act', ATTT[:, g8 * 8:g8 * 8 + nb, :],
                    pT[:, 0:nb * 128].rearrange("p (a b) -> p a b", b=128), [psb[tk]], [ATTTb])

        def s6(u):
            hp, tb, j = u['hp'], u['tb'], u['j']
            h = hpd[hp]
            po, pob = pst[6 + (tb % 2)], psb[6 + (tb % 2)]
            for sbk in range(tb + 1):
                MM(po[:, j * 64:(j + 1) * 64], u['ATTT'][:, sbk, :], h['v2'][:, sbk, j * 64:(j + 1) * 64],
                   [u['ATTTb'], h['v2b']], [pob], start=(sbk == 0), stop=(sbk == tb))

        def s7(u):
            hp, tb, j = u['hp'], u['tb'], u['j']
            if j == 0:
                return
            h = hpd[hp]
            oT, oTb = h['oT'], h['oTb']
            po, pob = pst[6 + (tb % 2)], psb[6 + (tb % 2)]
            O2, O2b = o2r.next()
            ACT(O2[:], po[:, 0:128], AF.Copy, [pob], [O2b])
            tk = tbank()
            pT2 = pst[tk][:].bitcast(BF16)
            TR(pT2[:, 0:128], O2[:], [O2b], [psb[tk]])
            CPY('dve', oT[:, tb * 128:(tb + 1) * 128], pT2[:, 0:128], [psb[tk]], [oTb])
            if tb == NQB - 1:
                for tt in range(NT):
                    ts = slice(tt * 512, (tt + 1) * 512)
                    sq, sqb = sqr.next()
                    ACT(sq[:], oT[:, ts], AF.Square, [oTb], [sqb])
                    tk = tbank()
                    MM(pst[tk][:], BONES64, sq[:], [sqb, cbfb], [psb[tk]])
                    rs, rsb = rsr.next()
                    ACT(rs[:], pst[tk][:], AF.Sqrt, [psb[tk], cstb], [rsb], bias=C_EPS6, scale=1.0)
                    RECIP(rs[:], rs[:], [rsb], [rsb])
                    yt, yb = yr.next()
                    STT(yt[:], oT[:, ts], V(l, V_SBG + hp), rs[:], ALU.mult, ALU.mult, [oTb, rsb, vecb], [yb])
                    P.dma('sp', mixT_s[1024 + hp * 128:1024 + (hp + 1) * 128, ts], yt[:], [yb], [DB('mixT', 8 + hp)], yb)

        stages = [s0, s1, s2, s3, s4, s5, s6, s7]
        units = [dict(hp=hp, tb=tb, j=j) for hp in range(8) for tb in range(NQB) for j in range(2)]
        NS = len(stages)
        for i in range(len(units) + NS - 1):
            for k in reversed(range(NS)):
                ui = i - k
                if 0 <= ui < len(units):
                    stages[k](units[ui])

    def phase_R(st, l, s):
        LW = sb("R_lw", [64, T], BF16, st)
        LA = sb("R_la", [64, T], BF16, st)
        LG = sb("R_lg", [128, T], BF16, st)
        LG2 = sb("R_lg2", [32, T], BF16, st)
        LV = sb("R_lv", [32, T], BF16, st)
        lob = Buf("R_lo")
        wup = sb("R_wup", [64, DR], BF16, st)
        aup = sb("R_aup", [64, DR], BF16, st)
        gup = sb("R_gup", [128, DR], BF16, st)
        gup2 = sb("R_gup2", [32, DR], BF16, st)
        vup = sb("R_vup", [32, DR], BF16, st)
        lwb = Buf("R_wup")
        aub = Buf("R_aup")
        gub = Buf("R_gup")
        gub2 = Buf("R_gup2")
        vub = Buf("R_vup")
        P.dma('pool', wup[:], w_up[l], [], [lwb], lwb)
        P.dma('pool', aup[:], a_up[l], [], [aub], aub)
        P.dma('pool', gup[:], g_up[l][0:128, :], [], [gub], gub)
        P.dma('pool', gup2[:], g_up[l][128:160, :], [], [gub2], gub2)
        if l > 0:
            P.dma('pool', vup[:], v_up[l - 1], [], [vub], vub)
        ldr = Ring(st, nc, "R_ld", [128, 512], F32, 4)
        for tt in range(NT):
            ts = slice(tt * 512, (tt + 1) * 512)
            t1, b1 = ldr.next()
            P.dma('sp', t1[0:64, :], xsT_s[3072:3136, ts], [DB('xsT', 24)], [b1], b1)
            ACT(LW[:, ts], t1[0:64, :], AF.Tanh, [b1], [lob])
            t2, b2 = ldr.next()
            P.dma('sp', t2[0:64, :], xsT_s[3136:3200, ts], [DB('xsT', 24)], [b2], b2)
            ACT(LA[:, ts], t2[0:64, :], AF.Copy, [b2], [lob])
            t3, b3 = ldr.next()
            P.dma('sp', t3[:], xsT_s[3200:3328, ts], [DB('xsT', 25)], [b3], b3)
            ACT(LG[:, ts], t3[:], AF.Sigmoid, [b3], [lob])
            t4, b4 = ldr.next()
            P.dma('sp', t4[0:32, :], xsT_s[3328:3360, ts], [DB('xsT', 26)], [b4], b4)
            ACT(LG2[:, ts], t4[0:32, :], AF.Sigmoid, [b4], [lob])
            if l > 0:
                t5, b5 = ldr.next()
                P.dma('sp', t5[0:32, :], xsT_s[P_R0:P_R0 + 32, ts], [DB('xsT', 26)], [b5], b5)
                ACT(LV[:, ts], t5[0:32, :], AF.Copy, [b5], [lob])

        NG = 4
        BK = [sb("R_bk%d" % h, [128, 8, 256], BF16, st) for h in range(NG)]
        AR = [sb("R_ar%d" % h, [128, 8, 256], BF16, st) for h in range(NG)]
        VT = [sb("R_vt%d" % h, [128, 8, 128], BF16, st) for h in range(NG)]
        WC = [sb("R_wc%d" % h, [128, 8], F32, st) for h in range(NG)]
        BON = [sb("R_bon%d" % h, [128, 512], F32, st) for h in range(NG)]
        YT = [sb("R_yt%d" % h, [128, 512], F32, st) for h in range(NG)]
        SF = [sb("R_sf%d" % h, [128, 128], F32, st) for h in range(8)]
        SB_ = [sb("R_sb%d" % h, [128, 128], BF16, st) for h in range(8)]
        gb_ = [{k: Buf("R_%s%d" % (k, h)) for k in ('bk', 'ar', 'vt', 'wc', 'bon', 'yt')} for h in range(NG)]
        sfb = [Buf("R_sf%d" % h) for h in range(8)]
        sbb = [Buf("R_sb%d" % h) for h in range(8)]
        for h in range(NG):
            MEMSET('pool', BK[h][:], 0.0, [gb_[h]['bk']])
            MEMSET('pool', AR[h][:], 0.0, [gb_[h]['ar']])
            MEMSET('pool', VT[h][:], 0.0, [gb_[h]['vt']])
        for h in range(8):
            MEMSET('pool', SF[h][:], 0.0, [sfb[h]])
            MEMSET('pool', SB_[h][:], 0.0, [sbb[h]])
        f32r = Ring(st, nc, "R_f", [128, 512], F32, 14)
        b16r = Ring(st, nc, "R_h", [128, 512], BF16, 4)
        mr = Ring(st, nc, "R_m", [128, 512], BF16, 3 * NG)
        pr = Ring(st, nc, "R_p", [128, 256], BF16, 3 * NG)
        qr = Ring(st, nc, "R_q", [128, 128], BF16, 4 * NG)
        tkr = Ring(st, nc, "R_tk", [128, 384], BF16, 3 * NG)
        xr_ = Ring(st, nc, "R_x", [128, 128], BF16, 2 * NG)
        ur_ = Ring(st, nc, "R_u", [128, 128], BF16, 2 * NG)
        s0r = Ring(st, nc, "R_s0", [128, 128], F32, 2 * NG)
        yo = Ring(st, nc, "R_yo", [128, 512], BF16, 3)
        pctr = [0]

        def psn():
            k = pctr[0] % 8
            pctr[0] += 1
            return pst[k], psb[k]

        ectr = [0]

        def cp(out, in_, r, w):
            ectr[0] += 1
            CPY('act' if ectr[0] % 2 == 0 else 'dve', out, in_, r, w)

        for tt in range(NT):
            ts = slice(tt * 512, (tt + 1) * 512)
            for grp in range(8 // NG):
                for g in range(NG):
                    h = grp * NG + g
                    gb = gb_[g]
                    cs = slice(h * 128, (h + 1) * 128)
                    r_, rb = f32r.next()
                    k_, kb = f32r.next()
                    v_, vb = f32r.next()
                    P.dma('sp', r_[:], xsT_s[h * 128:(h + 1) * 128, ts], [DB('xsT', h)], [rb], rb)
                    P.dma('act', k_[:], xsT_s[1024 + h * 128:1024 + (h + 1) * 128, ts], [DB('xsT', 8 + h)], [kb], kb)
                    P.dma('sp', v_[:], xsT_s[2048 + h * 128:2048 + (h + 1) * 128, ts], [DB('xsT', 16 + h)], [vb], vb)
                    ps, pb = psn()
                    MM(ps[:], wup[:, cs], LW[:, ts], [lwb, lob], [pb])
                    sg, sgb = f32r.next()
                    ACT(sg[:], ps[:], AF.Sigmoid, [pb, vecb], [sgb], bias=V(l, V_W0 + h), scale=1.0)
                    cum, cumb = f32r.next()
                    SCAN(cum[:], RESET, sg[:], 0.0, [sgb, cfb], [cumb])
                    Wt, Wtb = f32r.next()
                    iW, iWb = f32r.next()
                    Wex, Wexb = f32r.next()
                    ACT(Wt[:], cum[:], AF.Exp, [cumb], [Wtb], scale=-LAM)
                    ACT(iW[:], cum[:], AF.Exp, [cumb], [iWb], scale=LAM)
                    TT('dve', sg[:], cum[:], sg[:], ALU.subtract, [cumb, sgb], [sgb])
                    ACT(Wex[:], sg[:], AF.Exp, [sgb], [Wexb], scale=-LAM)
                    CPY('act', WC[g][:], c3(Wt[:])[:, :, 63], [Wtb], [gb['wc']])
                    ps, pb = psn()
                    MM(ps[:], aup[:, cs], LA[:, ts], [aub, lob], [pb])
                    a_, ab = f32r.next()
                    ACT(a_[:], ps[:], AF.Sigmoid, [pb, vecb], [ab], bias=V(l, V_A0 + h), scale=1.0)
                    if l == 0:
                        P.dma('sp', vfT_s[s][h * 128:(h + 1) * 128, ts], v_[:], [vb], [DB('vf', s, h)], vb)
                    else:
                        ps, pb = psn()
                        MM(ps[:], vup[:, cs], LV[:, ts], [vub, lob], [pb])
                        sv, svb = f32r.next()
                        ACT(sv[:], ps[:], AF.Sigmoid, [pb, vecb], [svb], bias=V(l, V_V0 + h), scale=1.0)
                        vf, vfb = f32r.next()
                        P.dma('act', vf[:], vfT_s[s][h * 128:(h + 1) * 128, ts], [DB('vf', s, h)], [vfb], vfb)
                        TT('dve', vf[:], vf[:], v_[:], ALU.subtract, [vfb, vb], [vfb])
                        TT('dve', vf[:], vf[:], sv[:], ALU.mult, [vfb, svb], [vfb])
                        TT('dve', v_[:], v_[:], vf[:], ALU.add, [vfb, vb], [vb])
                    kk, kkb = f32r.next()
                    ACT(kk[:], k_[:], AF.Copy, [kb, vecb], [kkb], scale=V(l, V_KK + h))
                    sq, sqb = b16r.next()
                    ACT(sq[:], kk[:], AF.Square, [kkb], [sqb])
                    ps, pb = psn()
                    MM(ps[:], BONES, sq[:], [sqb, cbfb], [pb])
                    rs, rsb = f32r.next()
                    TS('dve', rs[:], ps[:], 1e-24, None, ALU.max, None, [pb], [rsb])
                    ACT(rs[:], rs[:], AF.Sqrt, [rsb], [rsb])
                    RECIP(rs[:], rs[:], [rsb], [rsb])
                    TT('dve', kk[:], kk[:], rs[:], ALU.mult, [kkb, rsb], [kkb])
                    km, kmb = f32r.next()
                    TS('dve', km[:], a_[:], -1.0, V(l, V_KA + h), ALU.add, ALU.mult, [ab, vecb], [kmb])
                    STT(km[:], km[:], 1.0, k_[:], ALU.add, ALU.mult, [kmb, kb], [kmb])
                    rk, rkb = b16r.next()
                    STT(rk[:], r_[:], V(l, V_RK + h), km[:], ALU.mult, ALU.mult, [rb, kmb, vecb], [rkb])
                    ps, pb = psn()
                    MM(ps[:], BONES, rk[:], [rkb, cbfb], [pb])
                    TT('dve', BON[g][:], ps[:], v_[:], ALU.mult, [pb, vb], [gb['bon']])
                    TT('dve', a_[:], a_[:], kk[:], ALU.mult, [ab, kkb], [ab])
                    for j in range(2):
                        pp = slice(64 * j, 64 * j + 64)
                        co = 64 * j
                        e1 = 'dve'
                        STT(AR[g][pp, :, co:co + 64], c3(kk[pp, :]), -1.0, c3(Wex[pp, :]), ALU.mult, ALU.mult,
                            [kkb, Wexb], [gb['ar']])
                        TT(e1, AR[g][pp, :, 128 + co:128 + co + 64], c3(r_[pp, :]), c3(Wt[pp, :]), ALU.mult,
                           [rb, Wtb], [gb['ar']])
                        TT(e1, BK[g][pp, :, co:co + 64], c3(a_[pp, :]), c3(iW[pp, :]), ALU.mult, [ab, iWb], [gb['bk']])
                        TT(e1, BK[g][pp, :, 128 + co:128 + co + 64], c3(km[pp, :]), c3(iW[pp, :]), ALU.mult,
                           [kmb, iWb], [gb['bk']])
                        ACT(VT[g][pp, :, co:co + 64], c3(v_[pp, :]), AF.Copy, [vb], [gb['vt']])
                def st1(n, stt):
                    for g in range(NG):
                        gb = gb_[g]
                        d = stt[g]
                        ps1, pb1 = psn()
                        MM(ps1[:, 0:256], BK[g][:, n, 0:128], AR[g][:, n, :], [gb['bk'], gb['ar']], [pb1])
                        MM(ps1[:, 256:512], BK[g][:, n, 128:256], AR[g][:, n, :], [gb['bk'], gb['ar']], [pb1])
                        M_, Mb = mr.next()
                        TT('dve', M_[:], ps1[:], MSK4, ALU.mult, [pb1, cfb], [Mb])
                        ps2, pb2 = psn()
                        MM(ps2[:, 0:128], AR[g][:, n, 0:128], BK[g][:, n, 0:128], [gb['bk'], gb['ar']], [pb2])
                        PP, PPb = pr.next()
                        TT('dve', PP[:, 128:256], ps2[:, 0:128], MSKT, ALU.mult, [pb2, cfb], [PPb])
                        Q_, Qb = qr.next()
                        TT('dve', Q_[:], M_[:, 0:128], IDENT, ALU.add, [Mb, cbfb], [Qb])
                        ps3, pb3 = psn()
                        pT = ps3[:].bitcast(BF16)
                        TR(pT[:, 0:128], BK[g][:, n, 0:128], [gb['bk']], [pb3])
                        TR(pT[:, 128:256], BK[g][:, n, 128:256], [gb['bk']], [pb3])
                        TR(pT[:, 256:384], VT[g][:, n, :], [gb['vt']], [pb3])
                        TK, TKb = tkr.next()
                        ACT(TK[:], pT[:, 0:384], AF.Copy, [pb3], [TKb])
                        d.update(M=M_, Mb=Mb, PP=PP, PPb=PPb, Q=Q_, Qb=Qb, TK=TK, TKb=TKb)

                def level(lev, stt):
                    for g in range(NG):
                        d = stt[g]
                        PP, PPb, Q_, Qb = d['PP'], d['PPb'], d['Q'], d['Qb']
                        ps, pb = psn()
                        if lev == 0:
                            MM(ps[:, 0:128], PP[:, 128:256], d['M'][:, 0:128], [PPb, d['Mb']], [pb])
                            MM(ps[:, 128:256], d['M'][:, 0:128], PP[:, 128:256], [PPb, d['Mb']], [pb])
                            PN, PNb = pr.next()
                            cp(PN[:], ps[:, 0:256], [pb], [PNb])
                            d['PP'], d['PPb'] = PN, PNb
                        elif lev < 5:
                            MM(ps[:, 256:384], PP[:, 128:256], Q_[:], [PPb, Qb], [pb])
                            MM(ps[:, 0:128], PP[:, 128:256], PP[:, 0:128], [PPb], [pb])
                            MM(ps[:, 128:256], PP[:, 0:128], PP[:, 128:256], [PPb], [pb])
                            QN, QNb = qr.next()
                            TT('dve', QN[:], ps[:, 256:384], Q_[:], ALU.add, [pb, Qb], [QNb])
                            PN, PNb = pr.next()
                            ACT(PN[:], ps[:, 0:256], AF.Copy, [pb], [PNb])
                            d['PP'], d['PPb'], d['Q'], d['Qb'] = PN, PNb, QN, QNb
                        else:
                            MM(ps[:, 0:128], PP[:, 128:256], Q_[:], [PPb, Qb], [pb])
                            QN, QNb = qr.next()
                            TT('dve', QN[:], ps[:, 0:128], Q_[:], ALU.add, [pb, Qb], [QNb])
                            d['Q'], d['Qb'] = QN, QNb

                def sq_X(n, stt):
                    for g in range(NG):
                        h = grp * NG + g
                        gb = gb_[g]
                        d = stt[g]
                        ps, pb = psn()
                        MM(ps[:, 0:128], AR[g][:, n, 0:128], SB_[h][:], [gb['ar'], sbb[h]], [pb], start=True, stop=False)
                        MM(ps[:, 0:128], d['M'][:, 256:384], d['TK'][:, 256:384], [d['Mb'], d['TKb']], [pb],
                           start=False, stop=True)
                        X_, Xb = xr_.next()
                        cp(X_[:], ps[:, 0:128], [pb], [Xb])
                        d['X'], d['Xb'] = X_, Xb

                def sq_U(n, stt):
                    for g in range(NG):
                        d = stt[g]
                        ps, pb = psn()
                        MM(ps[:, 0:128], d['Q'][:], d['X'][:], [d['Qb'], d['Xb']], [pb])
                        U_, Ub = ur_.next()
                        cp(U_[:], ps[:, 0:128], [pb], [Ub])
                        d['U'], d['Ub'] = U_, Ub

                def sq_Y(n, stt):
                    for g in range(NG):
                        h = grp * NG + g
                        gb = gb_[g]
                        d = stt[g]
                        ps, pb = psn()
                        MM(ps[:, 0:128], SB_[h][:], AR[g][:, n, 128:256], [gb['ar'], sbb[h]], [pb], start=True, stop=False)
                        MM(ps[:, 0:128], d['U'][:], d['M'][:, 128:256], [d['Ub'], d['Mb']], [pb], start=False, stop=False)
                        MM(ps[:, 0:128], d['TK'][:, 256:384], d['M'][:, 384:512], [d['TKb'], d['Mb']], [pb],
                           start=False, stop=True)
                        ACT(YT[g][0:64, n * 64:(n + 1) * 64], ps[0:64, 0:64], AF.Copy, [pb], [gb['yt']])
                        CPY('dve', YT[g][64:128, n * 64:(n + 1) * 64], ps[64:128, 64:128], [pb], [gb['yt']])

                def sq_S(n, stt):
                    for g in range(NG):
                        h = grp * NG + g
                        gb = gb_[g]
                        d = stt[g]
                        ps, pb = psn()
                        MM(ps[:, 0:128], d['TK'][:, 0:128], d['U'][:], [d['TKb'], d['Ub']], [pb], start=True, stop=False)
                        MM(ps[:, 0:128], d['TK'][:, 128:256], d['TK'][:, 256:384], [d['TKb']], [pb], start=False, stop=True)
                        S0, S0b = s0r.next()
                        TS('dve', S0[:], SF[h][:], WC[g][:, n:n + 1], None, ALU.mult, None, [sfb[h], gb['wc']], [S0b])
                        STT(SF[h][:], ps[:, 0:128], WC[g][:, n:n + 1], S0[:], ALU.mult, ALU.add,
                            [pb, S0b, gb['wc']], [sfb[h]])
                        ACT(SB_[h][:], SF[h][:], AF.Copy, [sfb[h]], [sbb[h]])

                NCK = 8 if rstop >= 2 else 0
                chs = [[dict() for _ in range(NG)] for _ in range(NCK + 1)]
                if NCK:
                    st1(0, chs[0])
                    for lev in range(6):
                        level(lev, chs[0])
                for n in range(NCK):
                    nx = n + 1 < NCK
                    if nx:
                        st1(n + 1, chs[n + 1])
                    sq_X(n, chs[n])
                    if nx:
                        level(0, chs[n + 1])
                        level(1, chs[n + 1])
                    sq_U(n, chs[n])
                    if nx:
                        level(2, chs[n + 1])
                        level(3, chs[n + 1])
                    sq_Y(n, chs[n])
                    sq_S(n, chs[n])
                    if nx:
                        level(4, chs[n + 1])
                        level(5, chs[n + 1])
                for g in range(NG if rstop >= 5 else 0):
                    h = grp * NG + g
                    gb = gb_[g]
                    cs = slice(h * 128, (h + 1) * 128)
                    yb16, yb16b = b16r.next()
                    ACT(yb16[:], YT[g][:], AF.Copy, [gb['yt']], [yb16b])
                    ps, pb = psn()
                    MM(ps[:], BONES64, yb16[:], [yb16b, cbfb], [pb])
                    dd, ddb = f32r.next()
                    TT('dve', dd[:], YT[g][:], ps[:], ALU.subtract, [gb['yt'], pb], [ddb])
                    sq, sqb = b16r.next()
                    ACT(sq[:], dd[:], AF.Square, [ddb], [sqb])
                    ps2, pb2 = psn()
                    MM(ps2[:], BONES64, sq[:], [sqb, cbfb], [pb2])
                    rs, rsb = f32r.next()
                    ACT(rs[:], ps2[:], AF.Sqrt, [pb2, cstb], [rsb], bias=C_EPSLN, scale=1.0)
                    RECIP(rs[:], rs[:], [rsb], [rsb])
                    TT('dve', dd[:], dd[:], rs[:], ALU.mult, [ddb, rsb], [ddb])
                    TS('dve', dd[:], dd[:], V(l, V_LNW + h), V(l, V_LNB + h), ALU.mult, ALU.add, [ddb, vecb], [ddb])
                    TT('dve', dd[:], dd[:], BON[g][:], ALU.add, [ddb, gb['bon']], [ddb])
                    ps3, pb3 = psn()
                    MM(ps3[:], gup[:, cs], LG[:, ts], [gub, lob], [pb3], start=True, stop=False)
                    MM(ps3[:], gup2[:, cs], LG2[:, ts], [gub2, lob], [pb3], start=False, stop=True)
                    yt, ytb = yo.next()
                    TT('dve', yt[:], ps3[:], dd[:], ALU.mult, [pb3, ddb], [ytb])
                    P.dma('sp', mixT_s[h * 128:(h + 1) * 128, ts], yt[:], [ytb], [DB('mixT', h)], ytb)

    def scoped(fn, *a):
        P.barrier()
        P.scope_begin()
        with ExitStack() as st:
            fn(st, *a)
        P.barrier()
        P.scope_end()

    def seg_in(st, l, s):
        hT = sb("hT", [128, 16, T], BF16, st)
        hTb = Buf("hT")
        with ExitStack() as st2:
            if l == 0:
                norm_pass(st2, 0, 0, l, V_PREMIX, xT_in[s], ('xin', s), None, None, None, hT, hTb)
            else:
                norm_pass(st2, l - 1, V_POSTMLP, l, V_PREMIX, xT_s[s], ('x', s), oT_s[s], xT_s[s], ('x', s), hT, hTb, ('oT', s))
        P.barrier()
        phase_A(st, l, s, hT, hTb)

    def seg_mid(st, l, s):
        hT = sb("hT", [128, 16, T], BF16, st)
        hTb = Buf("hT")
        load_rhsT(hT, hTb, mixT_s, 'mixT')
        phase_C(st, l, s, hT, hTb)

    def seg_ffn(st, l, s):
        hT = sb("hT", [128, 16, T], BF16, st)
        hTb = Buf("hT")
        src = xT_in[s] if l == 0 else xT_s[s]
        skey = ('xin', s) if l == 0 else ('x', s)
        with ExitStack() as st2:
            norm_pass(st2, l, V_POSTMIX, l, V_PREMLP, src, skey, oT_s[s], xT_s[s], ('x', s), hT, hTb, ('oT', s))
        P.barrier()
        phase_F1(st, l, hT, hTb)

    def seg_final(st, s):
        norm_pass(st, NL - 1, V_POSTMLP, 0, 0, xT_s[s], ('x', s), oT_s[s], outT[s], ('out', s), None, None, ('oT', s))

    ph = phases or "ARSCFGZ"
    for l in range(NL):
        for s in range(NSEQ):
            if 'A' in ph:
                scoped(seg_in, l, s)
            if 'R' in ph:
                scoped(phase_R, l, s)
            if 'S' in ph:
                scoped(phase_S, l)
            if 'C' in ph:
                scoped(seg_mid, l, s)
            if 'F' in ph:
                scoped(seg_ffn, l, s)
            if 'G' in ph:
                scoped(phase_F2, l, s)
            if l == NL - 1 and 'Z' in ph:
                scoped(seg_final, s)
    P.finish()
    P.emit(es)
    es.close()
    return nc


_CACHE = {}


def to_tiles(xs):
    return np.ascontiguousarray(np.transpose(xs, (0, 2, 1)))


def from_tiles(xt):
    return np.transpose(np.asarray(xt), (0, 2, 1))


def kernel(**inputs):
    B, T, _ = inputs['x'].shape
    NL = inputs['w_in'].shape[0]
    NCORES = 8
    NSEQ = B // NCORES
    key = (T, NL, NSEQ)
    if key not in _CACHE:
        _CACHE[key] = build(T, NL, NSEQ)
    nc = _CACHE[key]
    x = np.asarray(inputs['x'], np.float32)
    consts = make_consts()
    vecs = pack_vecs(inputs, NL)
    shared = {
        'consts': consts, 'vecs': vecs,
        'w_in': np.ascontiguousarray(inputs['w_in'], np.float32),
        'w_in_vres': np.ascontiguousarray(inputs['w_in_vres'], np.float32),
        'w_up': np.ascontiguousarray(inputs['w_up'], np.float32),
        'a_up': np.ascontiguousarray(inputs['a_up'], np.float32),
        'g_up': np.ascontiguousarray(inputs['g_up'], np.float32),
        'v_up': np.ascontiguousarray(inputs['v_up'], np.float32),
        'w_out': np.ascontiguousarray(inputs['w_out'], np.float32),
        'w_ff_up': np.ascontiguousarray(inputs['w_ff_up'], np.float32),
        'w_ff_down': np.ascontiguousarray(inputs['w_ff_down'], np.float32),
    }
    in_maps = []
    for c in range(NCORES):
        m = dict(shared)
        m['xT'] = np.ascontiguousarray(np.transpose(x[c * NSEQ:(c + 1) * NSEQ], (0, 2, 1)))
        in_maps.append(m)
    res = run_bass_kernel_spmd(nc, in_maps, core_ids=list(range(NCORES)))
    outs = [np.transpose(r['outT'], (0, 2, 1)) for r in res.results]
    return np.ascontiguousarray(np.concatenate(outs, axis=0)).astype(np.float32)
```

```python
import numpy as np
import os
from contextlib import ExitStack
import concourse.bass as bass
import concourse.mybir as mybir
from concourse.bass_utils import run_bass_kernel_spmd

F32 = mybir.dt.float32
BF16 = mybir.dt.bfloat16
AF = mybir.ActivationFunctionType
ALU = mybir.AluOpType

D = 2048
DR = 1024
P_R0 = 3360
P_IN = 6432
DFF = 8192
LAM = float(np.exp(-0.5))
ENG = ['pe', 'act', 'dve', 'pool', 'sp']
SAME_SYNC = ('act', 'dve', 'pool')
SAME_ALL = bool(int(os.environ.get('SAME_ALL', '0')))

C_ID, C_BONES, C_STRICT, C_INCL, C_STRICTT, C_SBMASK, C_RESET = 0, 128, 256, 384, 768, 896, 1024
NCONST = 1024 + 512
V_PREMIX, V_POSTMIX, V_PREMLP, V_POSTMLP = 0, 16, 32, 48
V_MU = 64
V_MUV = 91
V_W0, V_A0, V_KK, V_KA, V_LNW, V_LNB, V_V0, V_RK, V_SBG = 92, 100, 108, 116, 124, 132, 140, 148, 156
NV = 164


def make_consts():
    c = np.zeros((128, NCONST), np.float32)
    p = np.arange(128)
    c[:, C_ID:C_ID + 128] = np.eye(128, dtype=np.float32)
    same = (p[:, None] // 64) == (p[None, :] // 64)
    c[:, C_BONES:C_BONES + 128] = same
    i = p[:, None] % 64
    t = p[None, :] % 64
    c[:, C_STRICT:C_STRICT + 128] = same & (i < t)
    c[:, C_INCL:C_INCL + 128] = same & (i <= t)
    c[:, C_STRICT + 256:C_STRICT + 384] = same & (i < t)
    c[:, C_INCL + 256:C_INCL + 384] = same & (i <= t)
    c[:, C_STRICTT:C_STRICTT + 128] = same & (i > t)
    c[:, C_SBMASK:C_SBMASK + 128] = (p[None, :] < p[:, None])
    c[:, C_RESET:C_RESET + 512] = (np.arange(512)[None, :] % 64 != 0)
    return c


def pack_vecs(inp, nl):
    v = np.zeros((128, nl * NV), np.float32)

    def put(l, col, arr, n):
        a = np.zeros(n * 128, np.float32)
        arr = np.asarray(arr, np.float32).reshape(-1)
        a[:arr.size] = arr
        v[:, l * NV + col:l * NV + col + n] = a.reshape(n, 128).T

    for l in range(nl):
        put(l, V_PREMIX, inp['pre_mix_g'][l], 16)
        put(l, V_POSTMIX, inp['post_mix_g'][l], 16)
        put(l, V_PREMLP, inp['pre_mlp_g'][l], 16)
        put(l, V_POSTMLP, inp['post_mlp_g'][l], 16)
        put(l, V_MU, inp['mu'][l], 27)
        put(l, V_W0, inp['w0'][l], 8)
        put(l, V_A0, inp['a0'][l], 8)
        put(l, V_KK, inp['k_k'][l], 8)
        put(l, V_KA, inp['k_a'][l], 8)
        put(l, V_LNW, inp['lnx_w'][l], 8)
        put(l, V_LNB, inp['lnx_b'][l], 8)
        put(l, V_RK, inp['r_k'][l], 8)
        put(l, V_SBG, inp['sb_out_g'][l], 8)
        if l > 0:
            put(l, V_MUV, inp['mu_vres'][l - 1], 1)
            put(l, V_V0, inp['v0'][l - 1], 8)
    return v


class Buf:
    __slots__ = ('name', 'w', 'r', 'chan', 'excl')

    def __init__(self, name, excl=False):
        self.name = name
        self.w = None
        self.r = {}
        self.chan = None
        self.excl = excl


class Prog:
    def __init__(self, nc):
        self.nc = nc
        self.ins = {e: [] for e in ENG}
        self.chan_count = []
        self.pending = {e: None for e in ENG}
        self.last_tok = {}
        self.free_ch = []
        self.scope_ch = None

    def _chan(self, buf):
        if buf.chan is None:
            if self.free_ch:
                buf.chan = self.free_ch.pop()
            else:
                buf.chan = len(self.chan_count)
                self.chan_count.append(0)
            if self.scope_ch is not None:
                self.scope_ch.append(buf.chan)
        return buf.chan

    def scope_begin(self):
        self.scope_ch = []

    def scope_end(self):
        self.free_ch.extend(self.scope_ch)
        self.scope_ch = None

    def op(self, eng, meth, reads=(), writes=(), chan_buf=None, same=False, **kw):
        lst = self.ins[eng]
        deps = set()
        if any(b.excl for b in reads):
            writes = list(writes) + [b for b in reads if b.excl and b not in writes]
            reads = [b for b in reads if not b.excl]
        for b in reads:
            if b.w is not None:
                deps.add(b.w)
        ch0 = self._chan(chan_buf) if chan_buf is not None else None
        for b in writes:
            if b.w is not None:
                if not (b is chan_buf and b.w[0] == 'c' and b.w[1] == ch0):
                    deps.add(b.w)
            deps.update(b.r.values())
        if self.pending[eng] is not None:
            deps |= self.pending[eng]
            self.pending[eng] = None
        if chan_buf is not None:
            ch = self._chan(chan_buf)
            self.chan_count[ch] += 1
            tok = ('c', ch, self.chan_count[ch])
            key = ('c', ch)
        else:
            ch = None
            tok = (eng, len(lst))
            key = eng
        lst.append({'fn': meth, 'kw': kw, 'deps': deps, 'chan': ch, 'signal': False, 'same': same})
        self.last_tok[key] = tok
        for b in reads:
            b.r[key] = tok
        for b in writes:
            b.w = tok
            b.r = {}
        return tok

    def dma(self, eng, out, in_, reads, writes, chan_buf):
        return self.op(eng, 'dma_start', reads, writes, chan_buf, out=out, in_=in_)

    def barrier(self):
        allt = set(self.last_tok.values())
        for e in ENG:
            self.pending[e] = set(allt) | (self.pending[e] or set())

    def finish(self):
        self.barrier()
        self.op('sp', None)

    def emit(self, es):
        nc = self.nc
        comp = ['pe', 'act', 'dve', 'pool']
        for e in ENG:
            for rec in self.ins[e]:
                for tok in rec['deps']:
                    if tok[0] == 'c':
                        continue
                    e2, i2 = tok
                    if e2 == e and not (e in SAME_SYNC and (rec['chan'] is not None or SAME_ALL or rec['same'])):
                        continue
                    self.ins[e2][i2]['signal'] = True
        for e in comp:
            cnt = 0
            for rec in self.ins[e]:
                if rec['signal']:
                    cnt += 1
                    rec['sigval'] = cnt
        esem = {e: es.enter_context(nc.semaphore('s_' + e)) for e in comp}
        csem = [es.enter_context(nc.semaphore('c_%d' % i)) for i in range(len(self.chan_count))]
        block = es.enter_context(nc.Block())
        prog = self

        def run(e, eo):
            known = {}
            for rec in prog.ins[e]:
                waits = {}
                for tok in rec['deps']:
                    if tok[0] == 'c':
                        key = ('c', tok[1])
                        val = 16 * tok[2]
                        sem = csem[tok[1]]
                    else:
                        e2, i2 = tok
                        if e2 == e and not (e in SAME_SYNC and (rec['chan'] is not None or SAME_ALL or rec['same'])):
                            continue
                        key = e2
                        val = prog.ins[e2][i2]['sigval']
                        sem = esem[e2]
                    if known.get(key, 0) >= val:
                        continue
                    if key not in waits or waits[key][0] < val:
                        waits[key] = (val, sem)
                for key, (val, sem) in waits.items():
                    known[key] = val
                    eo.wait_ge(sem, val)
                if rec['fn'] is None:
                    continue
                ins = getattr(eo, rec['fn'])(**rec['kw'])
                if rec['chan'] is not None:
                    ins.then_inc(csem[rec['chan']], 16)
                elif rec['signal']:
                    ins.then_inc(esem[e], 1)

        @block.tensor
        def _(eo):
            run('pe', eo)

        @block.scalar
        def _(eo):
            run('act', eo)

        @block.vector
        def _(eo):
            run('dve', eo)

        @block.gpsimd
        def _(eo):
            run('pool', eo)

        @block.sync
        def _(eo):
            run('sp', eo)


_UID = [0]


def _uid():
    _UID[0] += 1
    return _UID[0]


class Ring:
    def __init__(self, es, nc, name, shape, dtype, n):
        u = _uid()
        self.t = [es.enter_context(nc.sbuf_tensor('%s_%d_%d' % (name, u, i), shape, dtype)) for i in range(n)]
        self.b = [Buf('%s%d' % (name, i)) for i in range(n)]
        self.i = 0

    def next(self):
        k = self.i % len(self.t)
        self.i += 1
        return self.t[k], self.b[k]


class PsRing:
    def __init__(self, tens, bufs):
        self.t = tens
        self.b = bufs
        self.i = 0

    def next(self):
        k = self.i % len(self.t)
        self.i += 1
        return self.t[k], self.b[k]


def build(T, NL, NSEQ, debug=False, phases=None, rstop=99):
    assert T % 512 == 0
    NT = T // 512
    NQB = T // 128
    HT = min(1024, T)
    nc = bass.Bass("TRN2", target_bir_lowering=False)
    es = ExitStack()
    P = Prog(nc)

    def din(name, shape, dt=F32):
        return nc.dram_tensor(name, list(shape), dt, kind="ExternalInput").ap()

    def dscr(name, shape, dt=F32):
        kind = "ExternalOutput" if debug else "Internal"
        return nc.dram_tensor(name, list(shape), dt, kind=kind).ap()

    xT_in = din("xT", [NSEQ, D, T])
    consts_d = din("consts", [128, NCONST])
    vecs_d = din("vecs", [128, NL * NV])
    w_in = din("w_in", [NL, D, P_IN])
    w_in_vres = din("w_in_vres", [max(NL - 1, 1), D, 32])
    w_up = din("w_up", [NL, 64, DR])
    a_up = din("a_up", [NL, 64, DR])
    g_up = din("g_up", [NL, 160, DR])
    v_up = din("v_up", [max(NL - 1, 1), 32, DR])
    w_out = din("w_out", [NL, D, D])
    w_ff_up = din("w_ff_up", [NL, D, DFF])
    w_ff_down = din("w_ff_down", [NL, DFF, D])
    outT = nc.dram_tensor("outT", [NSEQ, D, T], F32, kind="ExternalOutput").ap()

    xT_s = dscr("xT_s", [NSEQ, D, T])
    oT_s = dscr("oT_s", [NSEQ, D, T])
    xsT_s = dscr("xsT_s", [3392, T])
    qT_s = dscr("qT_s", [1024, T], BF16)
    kT_s = dscr("kT_s", [1024, T], BF16)
    vtok_s = dscr("vtok_s", [T, 1024], BF16)
    mixT_s = dscr("mixT_s", [D, T], BF16)
    ffT_s = dscr("ffT_s", [DFF, T], BF16)
    vfT_s = dscr("vfT_s", [NSEQ, DR, T])

    dbufs = {}

    def DB(*key):
        if key not in dbufs:
            dbufs[key] = Buf(str(key))
        return dbufs[key]

    def sb(name, shape, dt, stack=None):
        return (stack or es).enter_context(nc.sbuf_tensor('%s_%d' % (name, _uid()), list(shape), dt))

    def ACT(out, in_, func, r, w, **kw):
        P.op('act', 'activation', r, w, out=out, in_=in_, func=func, **kw)

    def MM(out, lhsT, rhs, r, w, start=True, stop=True):
        P.op('pe', 'matmul', r, w, out=out, lhsT=lhsT, rhs=rhs, start=start, stop=stop)

    def TR(out, in_, r, w):
        P.op('pe', 'transpose', r + [cbfb], w, out=out, in_=in_, identity=IDENT)

    def TT(eng, out, in0, in1, op, r, w):
        P.op(eng, 'tensor_tensor', r, w, out=out, in0=in0, in1=in1, op=op)

    def TS(eng, out, in0, s1, s2, op0, op1, r, w):
        if s2 is None:
            P.op(eng, 'tensor_scalar', r, w, out=out, in0=in0, scalar1=s1, scalar2=None, op0=op0)
        else:
            P.op(eng, 'tensor_scalar', r, w, out=out, in0=in0, scalar1=s1, scalar2=s2, op0=op0, op1=op1)

    def STT(out, in0, scalar, in1, op0, op1, r, w):
        P.op('dve', 'scalar_tensor_tensor', r, w, out=out, in0=in0, scalar=scalar, in1=in1, op0=op0, op1=op1)

    def CPY(eng, out, in_, r, w):
        if eng == 'act':
            ACT(out, in_, AF.Copy, r, w)
        else:
            P.op(eng, 'tensor_copy', r, w, out=out, in_=in_)

    def RECIP(out, in_, r, w):
        P.op('dve', 'reciprocal', r, w, out=out, in_=in_)

    def MEMSET(eng, ap, val, w):
        P.op(eng, 'memset', [], w, ap=ap, constant=val)

    def SCAN(out, d0, d1, init, r, w):
        P.op('dve', 'tensor_tensor_scan', r, w, same=not isinstance(init, float), out=out, data0=d0, data1=d1,
             initial=init, op0=ALU.mult, op1=ALU.add)

    cf = sb("cf", [128, NCONST], F32)
    cfb = Buf("cf")
    vec = sb("vec", [128, NL * NV], F32)
    vecb = Buf("vec")
    omu = sb("omu", [128, NL * 28], F32)
    cbf = sb("cbf", [128, 4 * 128], BF16)
    cbfb = Buf("cbf")
    cst = sb("cst", [128, 8], F32)
    cstb = Buf("cst")
    ones_f = sb("ones_f", [128, 1024], F32)
    onesfb = Buf("onesf")

    P.dma('sp', cf[:], consts_d[:, :], [], [cfb], cfb)
    P.dma('sp', vec[:], vecs_d[:, :], [], [vecb], vecb)
    IDENT = cbf[:, 0:128]
    ONESB = cbf[:, 128:256]
    BONES = cbf[:, 256:384]
    BONES64 = cbf[:, 384:512]
    CPY('dve', cbf[:, 0:128], cf[:, C_ID:C_ID + 128], [cfb], [cbfb])
    MEMSET('dve', cbf[:, 128:256], 1.0, [cbfb])
    CPY('dve', cbf[:, 256:384], cf[:, C_BONES:C_BONES + 128], [cfb], [cbfb])
    TS('dve', cbf[:, 384:512], cf[:, C_BONES:C_BONES + 128], 1.0 / 64, None, ALU.mult, None, [cfb], [cbfb])
    MEMSET('dve', cst[:, 0:1], 1.0, [cstb])
    MEMSET('dve', cst[:, 1:2], 1e-6, [cstb])
    MEMSET('dve', cst[:, 2:3], 64e-5, [cstb])
    MEMSET('dve', ones_f[:], 1.0, [onesfb])
    for l in range(NL):
        TS('dve', omu[:, l * 28:l * 28 + 28], vec[:, l * NV + V_MU:l * NV + V_MU + 28], -1.0, 1.0, ALU.mult, ALU.add,
           [vecb], [vecb])
    C_ONE = cst[:, 0:1]
    C_EPS6 = cst[:, 1:2]
    C_EPSLN = cst[:, 2:3]
    MSK4 = cf[:, C_STRICT:C_STRICT + 512]
    MSKT = cf[:, C_STRICTT:C_STRICTT + 128]
    SBM = cf[:, C_SBMASK:C_SBMASK + 128]
    RESET = cf[:, C_RESET:C_RESET + 512]

    def V(l, col, n=1):
        return vec[:, l * NV + col:l * NV + col + n]

    pst = [es.enter_context(nc.psum_tensor('ps%d' % i, [128, 512], F32)) for i in range(8)]
    psb = [Buf('ps%d' % i, excl=True) for i in range(8)]

    def xview(ap2d):
        return ap2d.rearrange("(kc p) t -> p kc t", p=128)

    def c3(ap):
        return ap.rearrange("p (n c) -> p n c", c=64)

    NW = 256

    def rms_rstd(src3, srcb, sqr, rsr, psr):
        ps, pb = psr.next()
        for kc in range(16):
            sq, sqb = sqr.next()
            ACT(sq[:], src3[:, kc, :], AF.Square, [srcb], [sqb])
            MM(ps[:, 0:NW], ONESB, sq[:], [sqb, cbfb], [pb], start=(kc == 0), stop=(kc == 15))
        rs, rsb = rsr.next()
        ACT(rs[:], ps[:, 0:NW], AF.Sqrt, [pb, cstb], [rsb], bias=C_EPS6, scale=1.0 / D)
        RECIP(rs[:], rs[:], [rsb], [rsb])
        return rs, rsb

    def norm_pass(st, l_post, gpost_col, l_pre, gpre_col, src_x, src_key, o_ap, dst_x, dst_key, hT, hTb, o_key=None):
        xr = Ring(st, nc, "np_x", [128, 16, NW], F32, 2)
        orr = Ring(st, nc, "np_o", [128, 16, NW], F32, 2) if o_ap is not None else None
        sqr = Ring(st, nc, "np_sq", [128, NW], BF16, 4)
        rsr = Ring(st, nc, "np_rs", [128, NW], F32, 3)
        psr = PsRing(pst[0:2], psb[0:2])
        for t2 in range(T // NW):
            tt = (t2 * NW) // 512
            ts = slice(t2 * NW, (t2 + 1) * NW)
            xt, xb = xr.next()
            for q4 in range(2):
                P.dma('sp', xt[:, q4 * 8:(q4 + 1) * 8, :], xview(src_x)[:, q4 * 8:(q4 + 1) * 8, ts],
                      [DB(src_key, tt)], [xb], xb)
            if o_ap is not None:
                ot, ob = orr.next()
                for q4 in range(2):
                    P.dma('act', ot[:, q4 * 8:(q4 + 1) * 8, :], xview(o_ap)[:, q4 * 8:(q4 + 1) * 8, ts],
                          [DB(o_key, tt)], [ob], ob)
                rs, rsb = rms_rstd(ot, ob, sqr, rsr, psr)
                for kc in range(16):
                    STT(ot[:, kc, :], ot[:, kc, :], V(l_post, gpost_col + kc), rs[:], ALU.mult, ALU.mult,
                        [ob, rsb, vecb], [ob])
                    TT('dve', xt[:, kc, :], xt[:, kc, :], ot[:, kc, :], ALU.add, [ob, xb], [xb])
                if dst_x is not None:
                    for q4 in range(2):
                        P.dma('sp', xview(dst_x)[:, q4 * 8:(q4 + 1) * 8, ts], xt[:, q4 * 8:(q4 + 1) * 8, :],
                              [xb], [DB(dst_key, tt)], xb)
            if hT is not None:
                rs, rsb = rms_rstd(xt, xb, sqr, rsr, psr)
                for kc in range(16):
                    STT(hT[:, kc, ts], xt[:, kc, :], V(l_pre, gpre_col + kc), rs[:], ALU.mult, ALU.mult,
                        [xb, rsb, vecb], [hTb])

    def dense_fm(wr, rhsT, rhsb, groups, evac):
        psr = PsRing(pst[2:8], psb[2:8])
        for (W, f0, n, chunks) in groups:
            wt, wb = wr.next()
            Wv = W.rearrange("(kc p) f -> p kc f", p=128)
            for q4 in range(4):
                P.dma('pool', wt[:, q4 * 4:(q4 + 1) * 4, 0:n], Wv[:, q4 * 4:(q4 + 1) * 4, f0:f0 + n], [], [wb], wb)
            for (coff, fsz, info) in chunks:
                for tt in range(NT):
                    ps, pb = psr.next()
                    for kc in range(16):
                        MM(ps[0:fsz, :], wt[:, kc, coff:coff + fsz], rhsT[:, kc, tt * 512:(tt + 1) * 512],
                           [wb, rhsb], [pb], start=(kc == 0), stop=(kc == 15))
                    evac(info, fsz, tt, ps, pb)

    def phase_A(st, l, s, hT, hTb):
        wr = Ring(st, nc, "A_w", [128, 16, 512], BF16, 2)
        stA = Ring(st, nc, "A_a", [128, T], F32, 2)
        stB = Ring(st, nc, "A_b", [128, T + 1], F32, 2)
        stQ = Ring(st, nc, "A_q", [128, T], BF16, 2)
        stV = Ring(st, nc, "A_v", [128, 512], BF16, 3)
        for t_, b_ in zip(stB.t, stB.b):
            MEMSET('pool', t_[:, 0:1], 0.0, [b_])
        cur = {}
        W = w_in[l]

        def evac(info, fsz, tt, ps, pb):
            kind, idx = info
            ts = slice(tt * 512, (tt + 1) * 512)
            if kind in ('r', 'vres'):
                if tt == 0:
                    cur['A'] = stA.next()
                    cur['B'] = stB.next()
                (At, Ab), (Bt, Bb) = cur['A'], cur['B']
                if kind == 'r':
                    mu_ap = V(l, V_MU + idx)
                    omu_ap = omu[:, l * 28 + idx:l * 28 + idx + 1]
                    row0 = idx * 128
                else:
                    mu_ap = V(l, V_MUV)
                    omu_ap = omu[:, l * 28 + 27:l * 28 + 28]
                    row0 = P_R0
                ACT(At[0:fsz, ts], ps[0:fsz, :], AF.Copy, [pb, vecb], [Ab], scale=omu_ap[0:fsz, :])
                ACT(Bt[0:fsz, 1 + tt * 512:1 + (tt + 1) * 512], ps[0:fsz, :], AF.Copy, [pb, vecb], [Bb],
                    scale=mu_ap[0:fsz, :])
                if tt == NT - 1:
                    TT('dve', At[0:fsz, :], At[0:fsz, :], Bt[0:fsz, 0:T], ALU.add, [Ab, Bb], [Ab])
                    P.dma('sp', xsT_s[row0:row0 + fsz, :], At[0:fsz, :], [Ab], [DB('xsT', row0 // 128)], Ab)
            else:
                if tt == 0:
                    cur['Q'] = stQ.next()
                Qt, Qb = cur['Q']
                sc = 0.125 if kind == 'q' else 1.0
                ACT(Qt[:, ts], ps[:], AF.Copy, [pb], [Qb], scale=sc)
                if tt == NT - 1:
                    dst = qT_s if kind == 'q' else kT_s
                    P.dma('sp', dst[idx * 128:(idx + 1) * 128, :], Qt[:], [Qb], [DB(kind + 'T', idx)], Qb)

        groups = []
        for g in range(6):
            groups.append((W, g * 512, 512, [(c * 128, 128, ('r', g * 4 + c)) for c in range(4)]))
        groups.append((W, 3072, 288, [(0, 128, ('r', 24)), (128, 128, ('r', 25)), (256, 32, ('r', 26))]))
        for g in range(2):
            groups.append((W, P_R0 + g * 512, 512, [(c * 128, 128, ('q', g * 4 + c)) for c in range(4)]))
        for g in range(2):
            groups.append((W, P_R0 + 1024 + g * 512, 512, [(c * 128, 128, ('k', g * 4 + c)) for c in range(4)]))
        if l > 0:
            groups.append((w_in_vres[l - 1], 0, 32, [(0, 32, ('vres', 0))]))
        dense_fm(wr, hT, hTb, groups, evac)

        psr = PsRing(pst[2:8], psb[2:8])
        Wv = W.rearrange("(kc p) f -> p kc f", p=128)
        for g in range(2):
            wt, wb = wr.next()
            f0 = P_R0 + 2048 + g * 512
            for q4 in range(4):
                P.dma('pool', wt[:, q4 * 4:(q4 + 1) * 4, :], Wv[:, q4 * 4:(q4 + 1) * 4, f0:f0 + 512], [], [wb], wb)
            for tb in range(NQB):
                ps, pb = psr.next()
                for kc in range(16):
                    MM(ps[:], hT[:, kc, tb * 128:(tb + 1) * 128], wt[:, kc, :], [wb, hTb], [pb],
                       start=(kc == 0), stop=(kc == 15))
                vt, vb = stV.next()
                ACT(vt[:], ps[:], AF.Copy, [pb], [vb])
                P.dma('sp', vtok_s[tb * 128:(tb + 1) * 128, g * 512:(g + 1) * 512], vt[:], [vb],
                      [DB('vtok', tb // 4)], vb)

    def load_rhsT(hT, hTb, src, key):
        for q4 in range(4):
            for tt in range(NT):
                P.dma('sp', hT[:, q4 * 4:(q4 + 1) * 4, tt * 512:(tt + 1) * 512],
                      xview(src)[:, q4 * 4:(q4 + 1) * 4, tt * 512:(tt + 1) * 512],
                      [DB(key, kc) for kc in range(q4 * 4, q4 * 4 + 4)], [hTb], hTb)

    def phase_C(st, l, s, hT, hTb):
        wr = Ring(st, nc, "C_w", [128, 16, 512], BF16, 2)
        stO = Ring(st, nc, "C_o", [128, T], F32, 2)
        cur = {}

        def evac(info, fsz, tt, ps, pb):
            if tt == 0:
                cur['O'] = stO.next()
            Ot, Ob = cur['O']
            ACT(Ot[:, tt * 512:(tt + 1) * 512], ps[:], AF.Copy, [pb], [Ob])
            if tt == NT - 1:
                P.dma('sp', oT_s[s][info * 128:(info + 1) * 128, :], Ot[:], [Ob], [DB(('oT', s), t_) for t_ in range(NT)], Ob)

        groups = [(w_out[l], g * 512, 512, [(c * 128, 128, g * 4 + c) for c in range(4)]) for g in range(4)]
        dense_fm(wr, hT, hTb, groups, evac)

    def phase_F1(st, l, hT, hTb):
        wr = Ring(st, nc, "F1_w", [128, 16, 512], BF16, 2)
        stR = Ring(st, nc, "F_r", [128, 512], F32, 3)
        stF = Ring(st, nc, "F_f", [128, T], BF16, 2)
        cur = {}

        def evac(info, fsz, tt, ps, pb):
            if tt == 0:
                cur['F'] = stF.next()
            Ft, Fb = cur['F']
            rt, rb = stR.next()
            ACT(rt[:], ps[:], AF.Relu, [pb], [rb])
            if tt % 2 == 0:
                TT('dve', Ft[:, tt * 512:(tt + 1) * 512], rt[:], rt[:], ALU.mult, [rb], [Fb])
            else:
                ACT(Ft[:, tt * 512:(tt + 1) * 512], rt[:], AF.Square, [rb], [Fb])
            if tt == NT - 1:
                P.dma('sp', ffT_s[info * 128:(info + 1) * 128, :], Ft[:], [Fb], [DB('ffT', info)], Fb)

        groups = [(w_ff_up[l], g * 512, 512, [(c * 128, 128, g * 4 + c) for c in range(4)]) for g in range(16)]
        dense_fm(wr, hT, hTb, groups, evac)

    def phase_F2(st, l, s):
        ffh = sb("F2_ff", [128, 64, HT], BF16, st)
        ffq = [Buf("F2_ff%d" % q) for q in range(4)]
        wr = Ring(st, nc, "F2_w", [128, 64, 128], BF16, 2)
        stO = Ring(st, nc, "F2_o", [128, HT], F32, 2)
        NJ = HT // 512
        bi = 0
        Wv = w_ff_down[l].rearrange("(kc p) f -> p kc f", p=128)
        fv = ffT_s.rearrange("(kc p) t -> p kc t", p=128)
        for half in range(T // HT):
            hs = slice(half * HT, (half + 1) * HT)
            for q in range(16):
                P.dma('sp' if q % 2 == 0 else 'act', ffh[:, q * 4:(q + 1) * 4, :], fv[:, q * 4:(q + 1) * 4, hs],
                      [DB('ffT', kc) for kc in range(q * 4, q * 4 + 4)], [ffq[q // 4]], ffq[q // 4])
            for fc in range(16):
                wt, wb = wr.next()
                for q in range(4):
                    P.dma('pool', wt[:, q * 16:(q + 1) * 16, :], Wv[:, q * 16:(q + 1) * 16, fc * 128:(fc + 1) * 128],
                          [], [wb], wb)
                pss = []
                for j in range(NJ):
                    pss.append((pst[bi % 8], psb[bi % 8]))
                    bi += 1
                for kc in range(64):
                    for j in range(NJ):
                        ps, pb = pss[j]
                        MM(ps[:], wt[:, kc, :], ffh[:, kc, j * 512:(j + 1) * 512], [wb, ffq[kc // 16]], [pb],
                           start=(kc == 0), stop=(kc == 63))
                Ot, Ob = stO.next()
                for j in range(NJ):
                    ps, pb = pss[j]
                    ACT(Ot[:, j * 512:(j + 1) * 512], ps[:], AF.Copy, [pb], [Ob])
                P.dma('sp', oT_s[s][fc * 128:(fc + 1) * 128, hs], Ot[:], [Ob],
                      [DB(('oT', s), t_) for t_ in range(half * NJ, (half + 1) * NJ)], Ob)

    def phase_S(st, l):
        q2r = Ring(st, nc, "S_q", [128, T], BF16, 2)
        k2r = Ring(st, nc, "S_k", [128, T], BF16, 2)
        v2r = Ring(st, nc, "S_v", [128, NQB, 128], BF16, 2)
        er = Ring(st, nc, "S_e", [128, T], F32, 5)
        spr = Ring(st, nc, "S_sp", [128, 1024], F32, 6)
        psr_ = Ring(st, nc, "S_ps", [128, T + 1], F32, 4)
        attr = Ring(st, nc, "S_att", [128, T], BF16, 3)
        attTr = Ring(st, nc, "S_attT", [128, NQB, 128], BF16, 3)
        ntr = Ring(st, nc, "S_nt", [128, 1], F32, 6)
        o2r = Ring(st, nc, "S_o2", [128, 128], BF16, 2)
        oTr = Ring(st, nc, "S_oT", [128, T], BF16, 2)
        sqr = Ring(st, nc, "S_sq", [128, 512], BF16, 2)
        rsr = Ring(st, nc, "S_rs", [128, 512], F32, 2)
        yr = Ring(st, nc, "S_y", [128, 512], BF16, 2)
        vv = vtok_s.rearrange("(sb s) c -> s sb c", s=128)
        cnt = {'z': 0, 't': 0, 'e': 0}
        hpd = {}

        def zpair():
            k = (cnt['z'] % 2) * 2
            cnt['z'] += 1
            return k

        def tbank():
            k = 4 + (cnt['t'] % 2)
            cnt['t'] += 1
            return k

        def segs(nk):
            return [(c0, min(1024, nk - c0)) for c0 in range(0, nk, 1024)]

        def s0(u):
            hp, tb, j = u['hp'], u['tb'], u['j']
            if tb == 0 and j == 0:
                q2, q2b = q2r.next()
                k2, k2b = k2r.next()
                v2, v2b = v2r.next()
                P.dma('sp', q2[:], qT_s[hp * 128:(hp + 1) * 128, :], [DB('qT', hp)], [q2b], q2b)
                P.dma('sp', k2[:], kT_s[hp * 128:(hp + 1) * 128, :], [DB('kT', hp)], [k2b], k2b)
                P.dma('act', v2[:], vv[:, :, hp * 128:(hp + 1) * 128], [DB('vtok', i) for i in range((NQB + 3) // 4)],
                      [v2b], v2b)
                oT, oTb = oTr.next()
                hpd[hp] = dict(q2=q2, q2b=q2b, k2=k2, k2b=k2b, v2=v2, v2b=v2b, oT=oT, oTb=oTb)
            h = hpd[hp]
            nk = (tb + 1) * 128
            u['nk'] = nk
            pbs = slice(64 * j, 64 * j + 64)
            u['z'] = []
            for (c0, n) in segs(nk):
                zb = zpair()
                for hb in range((n + 511) // 512):
                    nn = min(512, n - hb * 512)
                    MM(pst[zb + hb][:, 0:nn], h['q2'][pbs, tb * 128:(tb + 1) * 128],
                       h['k2'][pbs, c0 + hb * 512:c0 + hb * 512 + nn], [h['q2b'], h['k2b']], [psb[zb + hb]])
                u['z'].append(zb)

        def s1(u):
            nk = u['nk']
            E, Eb = er.next()
            u['E'], u['Eb'] = E, Eb
            u['SP'] = []
            for si, (c0, n) in enumerate(segs(nk)):
                zb = u['z'][si]
                for hb in range((n + 511) // 512):
                    nn = min(512, n - hb * 512)
                    ACT(E[:, c0 + hb * 512:c0 + hb * 512 + nn], pst[zb + hb][:, 0:nn], AF.Exp, [psb[zb + hb]], [Eb])
                SP, SPb = spr.next()
                ACT(SP[:, 0:n], E[:, c0:c0 + n], AF.Ln, [Eb, cstb], [SPb], bias=C_ONE, scale=1.0)
                u['SP'].append((SP, SPb))

        def s2(u):
            nk = u['nk']
            PS, PSb = psr_.next()
            u['PS'], u['PSb'] = PS, PSb
            MEMSET('pool', PS[:, 0:1], 0.0, [PSb])
            for si, (c0, n) in enumerate(segs(nk)):
                SP, SPb = u['SP'][si]
                if c0 + n == nk:
                    TT('pool', SP[:, n - 128:n], SP[:, n - 128:n], SBM, ALU.mult, [SPb, cfb], [SPb])
                SCAN(PS[:, 1 + c0:1 + c0 + n], ones_f[:, 0:n], SP[:, 0:n],
                     (PS[:, c0:c0 + 1] if c0 > 0 else 0.0), [SPb, onesfb, PSb], [PSb])

        def s3(u):
            nk = u['nk']
            PS, PSb = u['PS'], u['PSb']
            NTt, NTb = ntr.next()
            TS('pool', NTt[:], PS[:, nk:nk + 1], -1.0, None, ALU.mult, None, [PSb], [NTb])
            for (c0, n) in segs(nk):
                ACT(PS[:, c0:c0 + n], PS[:, c0:c0 + n], AF.Exp, [PSb, NTb], [PSb], bias=NTt[:], scale=1.0)

        def s4(u):
            nk = u['nk']
            ATT, ATTb = attr.next()
            u['ATT'], u['ATTb'] = ATT, ATTb
            cnt['e'] += 1
            nA = (nk // 512) * 128
            if nA > 0:
                TT('pool', ATT[:, 0:nA], u['E'][:, 0:nA], u['PS'][:, 0:nA], ALU.mult, [u['Eb'], u['PSb']], [ATTb])
            TT('dve', ATT[:, nA:nk], u['E'][:, nA:nk], u['PS'][:, nA:nk], ALU.mult, [u['Eb'], u['PSb']], [ATTb])
            TT('pool', ATT[:, nk - 128:nk], ATT[:, nk - 128:nk], SBM, ALU.mult, [ATTb, cfb], [ATTb])

        def s5(u):
            tb = u['tb']
            ATT, ATTb = u['ATT'], u['ATTb']
            ATTT, ATTTb = attTr.next()
            u['ATTT'], u['ATTTb'] = ATTT, ATTTb
            for g8 in range((tb + 8) // 8):
                nb = min(8, tb + 1 - g8 * 8)
                tk = tbank()
                pT = pst[tk][:].bitcast(BF16)
                for s8 in range(nb):
                    sbk = g8 * 8 + s8
                    TR(pT[:, s8 * 128:(s8 + 1) * 128], ATT[:, sbk * 128:(sbk + 1) * 128], [ATTb], [psb[tk]])
                CPY('act', ATTT[:, g8 * 8:g8 * 8 + nb, :],
                    pT[:, 0:nb * 128].rearrange("p (a b) -> p a b", b=128), [psb[tk]], [ATTTb])

        def s6(u):
            hp, tb, j = u['hp'], u['tb'], u['j']
            h = hpd[hp]
            po, pob = pst[6 + (tb % 2)], psb[6 + (tb % 2)]
            for sbk in range(tb + 1):
                MM(po[:, j * 64:(j + 1) * 64], u['ATTT'][:, sbk, :], h['v2'][:, sbk, j * 64:(j + 1) * 64],
                   [u['ATTTb'], h['v2b']], [pob], start=(sbk == 0), stop=(sbk == tb))

        def s7(u):
            hp, tb, j = u['hp'], u['tb'], u['j']
            if j == 0:
                return
            h = hpd[hp]
            oT, oTb = h['oT'], h['oTb']
            po, pob = pst[6 + (tb % 2)], psb[6 + (tb % 2)]
            O2, O2b = o2r.next()
            ACT(O2[:], po[:, 0:128], AF.Copy, [pob], [O2b])
            tk = tbank()
            pT2 = pst[tk][:].bitcast(BF16)
            TR(pT2[:, 0:128], O2[:], [O2b], [psb[tk]])
            CPY('dve', oT[:, tb * 128:(tb + 1) * 128], pT2[:, 0:128], [psb[tk]], [oTb])
            if tb == NQB - 1:
                for tt in range(NT):
                    ts = slice(tt * 512, (tt + 1) * 512)
                    sq, sqb = sqr.next()
                    ACT(sq[:], oT[:, ts], AF.Square, [oTb], [sqb])
                    tk = tbank()
                    MM(pst[tk][:], BONES64, sq[:], [sqb, cbfb], [psb[tk]])
                    rs, rsb = rsr.next()
                    ACT(rs[:], pst[tk][:], AF.Sqrt, [psb[tk], cstb], [rsb], bias=C_EPS6, scale=1.0)
                    RECIP(rs[:], rs[:], [rsb], [rsb])
                    yt, yb = yr.next()
                    STT(yt[:], oT[:, ts], V(l, V_SBG + hp), rs[:], ALU.mult, ALU.mult, [oTb, rsb, vecb], [yb])
                    P.dma('sp', mixT_s[1024 + hp * 128:1024 + (hp + 1) * 128, ts], yt[:], [yb], [DB('mixT', 8 + hp)], yb)

        stages = [s0, s1, s2, s3, s4, s5, s6, s7]
        units = [dict(hp=hp, tb=tb, j=j) for hp in range(8) for tb in range(NQB) for j in range(2)]
        NS = len(stages)
        for i in range(len(units) + NS - 1):
            for k in reversed(range(NS)):
                ui = i - k
                if 0 <= ui < len(units):
                    stages[k](units[ui])

    def phase_R(st, l, s):
        LW = sb("R_lw", [64, T], BF16, st)
        LA = sb("R_la", [64, T], BF16, st)
        LG = sb("R_lg", [128, T], BF16, st)
        LG2 = sb("R_lg2", [32, T], BF16, st)
        LV = sb("R_lv", [32, T], BF16, st)
        lob = Buf("R_lo")
        wup = sb("R_wup", [64, DR], BF16, st)
        aup = sb("R_aup", [64, DR], BF16, st)
        gup = sb("R_gup", [128, DR], BF16, st)
        gup2 = sb("R_gup2", [32, DR], BF16, st)
        vup = sb("R_vup", [32, DR], BF16, st)
        lwb = Buf("R_wup")
        aub = Buf("R_aup")
        gub = Buf("R_gup")
        gub2 = Buf("R_gup2")
        vub = Buf("R_vup")
        P.dma('pool', wup[:], w_up[l], [], [lwb], lwb)
        P.dma('pool', aup[:], a_up[l], [], [aub], aub)
        P.dma('pool', gup[:], g_up[l][0:128, :], [], [gub], gub)
        P.dma('pool', gup2[:], g_up[l][128:160, :], [], [gub2], gub2)
        if l > 0:
            P.dma('pool', vup[:], v_up[l - 1], [], [vub], vub)
        ldr = Ring(st, nc, "R_ld", [128, 512], F32, 4)
        for tt in range(NT):
            ts = slice(tt * 512, (tt + 1) * 512)
            t1, b1 = ldr.next()
            P.dma('sp', t1[0:64, :], xsT_s[3072:3136, ts], [DB('xsT', 24)], [b1], b1)
            ACT(LW[:, ts], t1[0:64, :], AF.Tanh, [b1], [lob])
            t2, b2 = ldr.next()
            P.dma('sp', t2[0:64, :], xsT_s[3136:3200, ts], [DB('xsT', 24)], [b2], b2)
            ACT(LA[:, ts], t2[0:64, :], AF.Copy, [b2], [lob])
            t3, b3 = ldr.next()
            P.dma('sp', t3[:], xsT_s[3200:3328, ts], [DB('xsT', 25)], [b3], b3)
            ACT(LG[:, ts], t3[:], AF.Sigmoid, [b3], [lob])
            t4, b4 = ldr.next()
            P.dma('sp', t4[0:32, :], xsT_s[3328:3360, ts], [DB('xsT', 26)], [b4], b4)
            ACT(LG2[:, ts], t4[0:32, :], AF.Sigmoid, [b4], [lob])
            if l > 0:
                t5, b5 = ldr.next()
                P.dma('sp', t5[0:32, :], xsT_s[P_R0:P_R0 + 32, ts], [DB('xsT', 26)], [b5], b5)
                ACT(LV[:, ts], t5[0:32, :], AF.Copy, [b5], [lob])

        NG = 4
        BK = [sb("R_bk%d" % h, [128, 8, 256], BF16, st) for h in range(NG)]
        AR = [sb("R_ar%d" % h, [128, 8, 256], BF16, st) for h in range(NG)]
        VT = [sb("R_vt%d" % h, [128, 8, 128], BF16, st) for h in range(NG)]
        WC = [sb("R_wc%d" % h, [128, 8], F32, st) for h in range(NG)]
        BON = [sb("R_bon%d" % h, [128, 512], F32, st) for h in range(NG)]
        YT = [sb("R_yt%d" % h, [128, 512], F32, st) for h in range(NG)]
        SF = [sb("R_sf%d" % h, [128, 128], F32, st) for h in range(8)]
        SB_ = [sb("R_sb%d" % h, [128, 128], BF16, st) for h in range(8)]
        gb_ = [{k: Buf("R_%s%d" % (k, h)) for k in ('bk', 'ar', 'vt', 'wc', 'bon', 'yt')} for h in range(NG)]
        sfb = [Buf("R_sf%d" % h) for h in range(8)]
        sbb = [Buf("R_sb%d" % h) for h in range(8)]
        for h in range(NG):
            MEMSET('pool', BK[h][:], 0.0, [gb_[h]['bk']])
            MEMSET('pool', AR[h][:], 0.0, [gb_[h]['ar']])
            MEMSET('pool', VT[h][:], 0.0, [gb_[h]['vt']])
        for h in range(8):
            MEMSET('pool', SF[h][:], 0.0, [sfb[h]])
            MEMSET('pool', SB_[h][:], 0.0, [sbb[h]])
        f32r = Ring(st, nc, "R_f", [128, 512], F32, 14)
        b16r = Ring(st, nc, "R_h", [128, 512], BF16, 4)
        mr = Ring(st, nc, "R_m", [128, 512], BF16, 3 * NG)
        pr = Ring(st, nc, "R_p", [128, 256], BF16, 3 * NG)
        qr = Ring(st, nc, "R_q", [128, 128], BF16, 4 * NG)
        tkr = Ring(st, nc, "R_tk", [128, 384], BF16, 3 * NG)
        xr_ = Ring(st, nc, "R_x", [128, 128], BF16, 2 * NG)
        ur_ = Ring(st, nc, "R_u", [128, 128], BF16, 2 * NG)
        s0r = Ring(st, nc, "R_s0", [128, 128], F32, 2 * NG)
        yo = Ring(st, nc, "R_yo", [128, 512], BF16, 3)
        pctr = [0]

        def psn():
            k = pctr[0] % 8
            pctr[0] += 1
            return pst[k], psb[k]

        ectr = [0]

        def cp(out, in_, r, w):
            ectr[0] += 1
            CPY('act' if ectr[0] % 2 == 0 else 'dve', out, in_, r, w)

        for tt in range(NT):
            ts = slice(tt * 512, (tt + 1) * 512)
            for grp in range(8 // NG):
                for g in range(NG):
                    h = grp * NG + g
                    gb = gb_[g]
                    cs = slice(h * 128, (h + 1) * 128)
                    r_, rb = f32r.next()
                    k_, kb = f32r.next()
                    v_, vb = f32r.next()
                    P.dma('sp', r_[:], xsT_s[h * 128:(h + 1) * 128, ts], [DB('xsT', h)], [rb], rb)
                    P.dma('act', k_[:], xsT_s[1024 + h * 128:1024 + (h + 1) * 128, ts], [DB('xsT', 8 + h)], [kb], kb)
                    P.dma('sp', v_[:], xsT_s[2048 + h * 128:2048 + (h + 1) * 128, ts], [DB('xsT', 16 + h)], [vb], vb)
                    ps, pb = psn()
                    MM(ps[:], wup[:, cs], LW[:, ts], [lwb, lob], [pb])
                    sg, sgb = f32r.next()
                    ACT(sg[:], ps[:], AF.Sigmoid, [pb, vecb], [sgb], bias=V(l, V_W0 + h), scale=1.0)
                    cum, cumb = f32r.next()
                    SCAN(cum[:], RESET, sg[:], 0.0, [sgb, cfb], [cumb])
                    Wt, Wtb = f32r.next()
                    iW, iWb = f32r.next()
                    Wex, Wexb = f32r.next()
                    ACT(Wt[:], cum[:], AF.Exp, [cumb], [Wtb], scale=-LAM)
                    ACT(iW[:], cum[:], AF.Exp, [cumb], [iWb], scale=LAM)
                    TT('dve', sg[:], cum[:], sg[:], ALU.subtract, [cumb, sgb], [sgb])
                    ACT(Wex[:], sg[:], AF.Exp, [sgb], [Wexb], scale=-LAM)
                    CPY('act', WC[g][:], c3(Wt[:])[:, :, 63], [Wtb], [gb['wc']])
                    ps, pb = psn()
                    MM(ps[:], aup[:, cs], LA[:, ts], [aub, lob], [pb])
                    a_, ab = f32r.next()
                    ACT(a_[:], ps[:], AF.Sigmoid, [pb, vecb], [ab], bias=V(l, V_A0 + h), scale=1.0)
                    if l == 0:
                        P.dma('sp', vfT_s[s][h * 128:(h + 1) * 128, ts], v_[:], [vb], [DB('vf', s, h)], vb)
                    else:
                        ps, pb = psn()
                        MM(ps[:], vup[:, cs], LV[:, ts], [vub, lob], [pb])
                        sv, svb = f32r.next()
                        ACT(sv[:], ps[:], AF.Sigmoid, [pb, vecb], [svb], bias=V(l, V_V0 + h), scale=1.0)
                        vf, vfb = f32r.next()
                        P.dma('act', vf[:], vfT_s[s][h * 128:(h + 1) * 128, ts], [DB('vf', s, h)], [vfb], vfb)
                        TT('dve', vf[:], vf[:], v_[:], ALU.subtract, [vfb, vb], [vfb])
                        TT('dve', vf[:], vf[:], sv[:], ALU.mult, [vfb, svb], [vfb])
                        TT('dve', v_[:], v_[:], vf[:], ALU.add, [vfb, vb], [vb])
                    kk, kkb = f32r.next()
                    ACT(kk[:], k_[:], AF.Copy, [kb, vecb], [kkb], scale=V(l, V_KK + h))
                    sq, sqb = b16r.next()
                    ACT(sq[:], kk[:], AF.Square, [kkb], [sqb])
                    ps, pb = psn()
                    MM(ps[:], BONES, sq[:], [sqb, cbfb], [pb])
                    rs, rsb = f32r.next()
                    TS('dve', rs[:], ps[:], 1e-24, None, ALU.max, None, [pb], [rsb])
                    ACT(rs[:], rs[:], AF.Sqrt, [rsb], [rsb])
                    RECIP(rs[:], rs[:], [rsb], [rsb])
                    TT('dve', kk[:], kk[:], rs[:], ALU.mult, [kkb, rsb], [kkb])
                    km, kmb = f32r.next()
                    TS('dve', km[:], a_[:], -1.0, V(l, V_KA + h), ALU.add, ALU.mult, [ab, vecb], [kmb])
                    STT(km[:], km[:], 1.0, k_[:], ALU.add, ALU.mult, [kmb, kb], [kmb])
                    rk, rkb = b16r.next()
                    STT(rk[:], r_[:], V(l, V_RK + h), km[:], ALU.mult, ALU.mult, [rb, kmb, vecb], [rkb])
                    ps, pb = psn()
                    MM(ps[:], BONES, rk[:], [rkb, cbfb], [pb])
                    TT('dve', BON[g][:], ps[:], v_[:], ALU.mult, [pb, vb], [gb['bon']])
                    TT('dve', a_[:], a_[:], kk[:], ALU.mult, [ab, kkb], [ab])
                    for j in range(2):
                        pp = slice(64 * j, 64 * j + 64)
                        co = 64 * j
                        e1 = 'dve'
                        STT(AR[g][pp, :, co:co + 64], c3(kk[pp, :]), -1.0, c3(Wex[pp, :]), ALU.mult, ALU.mult,
                            [kkb, Wexb], [gb['ar']])
                        TT(e1, AR[g][pp, :, 128 + co:128 + co + 64], c3(r_[pp, :]), c3(Wt[pp, :]), ALU.mult,
                           [rb, Wtb], [gb['ar']])
                        TT(e1, BK[g][pp, :, co:co + 64], c3(a_[pp, :]), c3(iW[pp, :]), ALU.mult, [ab, iWb], [gb['bk']])
                        TT(e1, BK[g][pp, :, 128 + co:128 + co + 64], c3(km[pp, :]), c3(iW[pp, :]), ALU.mult,
                           [kmb, iWb], [gb['bk']])
                        ACT(VT[g][pp, :, co:co + 64], c3(v_[pp, :]), AF.Copy, [vb], [gb['vt']])
                def st1(n, stt):
                    for g in range(NG):
                        gb = gb_[g]
                        d = stt[g]
                        ps1, pb1 = psn()
                        MM(ps1[:, 0:256], BK[g][:, n, 0:128], AR[g][:, n, :], [gb['bk'], gb['ar']], [pb1])
                        MM(ps1[:, 256:512], BK[g][:, n, 128:256], AR[g][:, n, :], [gb['bk'], gb['ar']], [pb1])
                        M_, Mb = mr.next()
                        TT('dve', M_[:], ps1[:], MSK4, ALU.mult, [pb1, cfb], [Mb])
                        ps2, pb2 = psn()
                        MM(ps2[:, 0:128], AR[g][:, n, 0:128], BK[g][:, n, 0:128], [gb['bk'], gb['ar']], [pb2])
                        PP, PPb = pr.next()
                        TT('dve', PP[:, 128:256], ps2[:, 0:128], MSKT, ALU.mult, [pb2, cfb], [PPb])
                        Q_, Qb = qr.next()
                        TT('dve', Q_[:], M_[:, 0:128], IDENT, ALU.add, [Mb, cbfb], [Qb])
                        ps3, pb3 = psn()
                        pT = ps3[:].bitcast(BF16)
                        TR(pT[:, 0:128], BK[g][:, n, 0:128], [gb['bk']], [pb3])
                        TR(pT[:, 128:256], BK[g][:, n, 128:256], [gb['bk']], [pb3])
                        TR(pT[:, 256:384], VT[g][:, n, :], [gb['vt']], [pb3])
                        TK, TKb = tkr.next()
                        ACT(TK[:], pT[:, 0:384], AF.Copy, [pb3], [TKb])
                        d.update(M=M_, Mb=Mb, PP=PP, PPb=PPb, Q=Q_, Qb=Qb, TK=TK, TKb=TKb)

                def level(lev, stt):
                    for g in range(NG):
                        d = stt[g]
                        PP, PPb, Q_, Qb = d['PP'], d['PPb'], d['Q'], d['Qb']
                        ps, pb = psn()
                        if lev == 0:
                            MM(ps[:, 0:128], PP[:, 128:256], d['M'][:, 0:128], [PPb, d['Mb']], [pb])
                            MM(ps[:, 128:256], d['M'][:, 0:128], PP[:, 128:256], [PPb, d['Mb']], [pb])
                            PN, PNb = pr.next()
                            cp(PN[:], ps[:, 0:256], [pb], [PNb])
                            d['PP'], d['PPb'] = PN, PNb
                        elif lev < 5:
                            MM(ps[:, 256:384], PP[:, 128:256], Q_[:], [PPb, Qb], [pb])
                            MM(ps[:, 0:128], PP[:, 128:256], PP[:, 0:128], [PPb], [pb])
                            MM(ps[:, 128:256], PP[:, 0:128], PP[:, 128:256], [PPb], [pb])
                            QN, QNb = qr.next()
                            TT('dve', QN[:], ps[:, 256:384], Q_[:], ALU.add, [pb, Qb], [QNb])
                            PN, PNb = pr.next()
                            ACT(PN[:], ps[:, 0:256], AF.Copy, [pb], [PNb])
                            d['PP'], d['PPb'], d['Q'], d['Qb'] = PN, PNb, QN, QNb
                        else:
                            MM(ps[:, 0:128], PP[:, 128:256], Q_[:], [PPb, Qb], [pb])
                            QN, QNb = qr.next()
                            TT('dve', QN[:], ps[:, 0:128], Q_[:], ALU.add, [pb, Qb], [QNb])
                            d['Q'], d['Qb'] = QN, QNb

                def sq_X(n, stt):
                    for g in range(NG):
                        h = grp * NG + g
                        gb = gb_[g]
                        d = stt[g]
                        ps, pb = psn()
                        MM(ps[:, 0:128], AR[g][:, n, 0:128], SB_[h][:], [gb['ar'], sbb[h]], [pb], start=True, stop=False)
                        MM(ps[:, 0:128], d['M'][:, 256:384], d['TK'][:, 256:384], [d['Mb'], d['TKb']], [pb],
                           start=False, stop=True)
                        X_, Xb = xr_.next()
                        cp(X_[:], ps[:, 0:128], [pb], [Xb])
                        d['X'], d['Xb'] = X_, Xb

                def sq_U(n, stt):
                    for g in range(NG):
                        d = stt[g]
                        ps, pb = psn()
                        MM(ps[:, 0:128], d['Q'][:], d['X'][:], [d['Qb'], d['Xb']], [pb])
                        U_, Ub = ur_.next()
                        cp(U_[:], ps[:, 0:128], [pb], [Ub])
                        d['U'], d['Ub'] = U_, Ub

                def sq_Y(n, stt):
                    for g in range(NG):
                        h = grp * NG + g
                        gb = gb_[g]
                        d = stt[g]
                        ps, pb = psn()
                        MM(ps[:, 0:128], SB_[h][:], AR[g][:, n, 128:256], [gb['ar'], sbb[h]], [pb], start=True, stop=False)
                        MM(ps[:, 0:128], d['U'][:], d['M'][:, 128:256], [d['Ub'], d['Mb']], [pb], start=False, stop=False)
                        MM(ps[:, 0:128], d['TK'][:, 256:384], d['M'][:, 384:512], [d['TKb'], d['Mb']], [pb],
                           start=False, stop=True)
                        ACT(YT[g][0:64, n * 64:(n + 1) * 64], ps[0:64, 0:64], AF.Copy, [pb], [gb['yt']])
                        CPY('dve', YT[g][64:128, n * 64:(n + 1) * 64], ps[64:128, 64:128], [pb], [gb['yt']])

                def sq_S(n, stt):
                    for g in range(NG):
                        h = grp * NG + g
                        gb = gb_[g]
                        d = stt[g]
                        ps, pb = psn()
                        MM(ps[:, 0:128], d['TK'][:, 0:128], d['U'][:], [d['TKb'], d['Ub']], [pb], start=True, stop=False)
                        MM(ps[:, 0:128], d['TK'][:, 128:256], d['TK'][:, 256:384], [d['TKb']], [pb], start=False, stop=True)
                        S0, S0b = s0r.next()
                        TS('dve', S0[:], SF[h][:], WC[g][:, n:n + 1], None, ALU.mult, None, [sfb[h], gb['wc']], [S0b])
                        STT(SF[h][:], ps[:, 0:128], WC[g][:, n:n + 1], S0[:], ALU.mult, ALU.add,
                            [pb, S0b, gb['wc']], [sfb[h]])
                        ACT(SB_[h][:], SF[h][:], AF.Copy, [sfb[h]], [sbb[h]])

                NCK = 8 if rstop >= 2 else 0
                chs = [[dict() for _ in range(NG)] for _ in range(NCK + 1)]
                if NCK:
                    st1(0, chs[0])
                    for lev in range(6):
                        level(lev, chs[0])
                for n in range(NCK):
                    nx = n + 1 < NCK
                    if nx:
                        st1(n + 1, chs[n + 1])
                    sq_X(n, chs[n])
                    if nx:
                        level(0, chs[n + 1])
                        level(1, chs[n + 1])
                    sq_U(n, chs[n])
                    if nx:
                        level(2, chs[n + 1])
                        level(3, chs[n + 1])
                    sq_Y(n, chs[n])
                    sq_S(n, chs[n])
                    if nx:
                        level(4, chs[n + 1])
                        level(5, chs[n + 1])
                for g in range(NG if rstop >= 5 else 0):
                    h = grp * NG + g
                    gb = gb_[g]
                    cs = slice(h * 128, (h + 1) * 128)
                    yb16, yb16b = b16r.next()
                    ACT(yb16[:], YT[g][:], AF.Copy, [gb['yt']], [yb16b])
                    ps, pb = psn()
                    MM(ps[:], BONES64, yb16[:], [yb16b, cbfb], [pb])
                    dd, ddb = f32r.next()
                    TT('dve', dd[:], YT[g][:], ps[:], ALU.subtract, [gb['yt'], pb], [ddb])
                    sq, sqb = b16r.next()
                    ACT(sq[:], dd[:], AF.Square, [ddb], [sqb])
                    ps2, pb2 = psn()
                    MM(ps2[:], BONES64, sq[:], [sqb, cbfb], [pb2])
                    rs, rsb = f32r.next()
                    ACT(rs[:], ps2[:], AF.Sqrt, [pb2, cstb], [rsb], bias=C_EPSLN, scale=1.0)
                    RECIP(rs[:], rs[:], [rsb], [rsb])
                    TT('dve', dd[:], dd[:], rs[:], ALU.mult, [ddb, rsb], [ddb])
                    TS('dve', dd[:], dd[:], V(l, V_LNW + h), V(l, V_LNB + h), ALU.mult, ALU.add, [ddb, vecb], [ddb])
                    TT('dve', dd[:], dd[:], BON[g][:], ALU.add, [ddb, gb['bon']], [ddb])
                    ps3, pb3 = psn()
                    MM(ps3[:], gup[:, cs], LG[:, ts], [gub, lob], [pb3], start=True, stop=False)
                    MM(ps3[:], gup2[:, cs], LG2[:, ts], [gub2, lob], [pb3], start=False, stop=True)
                    yt, ytb = yo.next()
                    TT('dve', yt[:], ps3[:], dd[:], ALU.mult, [pb3, ddb], [ytb])
                    P.dma('sp', mixT_s[h * 128:(h + 1) * 128, ts], yt[:], [ytb], [DB('mixT', h)], ytb)

    def scoped(fn, *a):
        P.barrier()
        P.scope_begin()
        with ExitStack() as st:
            fn(st, *a)
        P.barrier()
        P.scope_end()

    def seg_in(st, l, s):
        hT = sb("hT", [128, 16, T], BF16, st)
        hTb = Buf("hT")
        with ExitStack() as st2:
            if l == 0:
                norm_pass(st2, 0, 0, l, V_PREMIX, xT_in[s], ('xin', s), None, None, None, hT, hTb)
            else:
                norm_pass(st2, l - 1, V_POSTMLP, l, V_PREMIX, xT_s[s], ('x', s), oT_s[s], xT_s[s], ('x', s), hT, hTb, ('oT', s))
        P.barrier()
        phase_A(st, l, s, hT, hTb)

    def seg_mid(st, l, s):
        hT = sb("hT", [128, 16, T], BF16, st)
        hTb = Buf("hT")
        load_rhsT(hT, hTb, mixT_s, 'mixT')
        phase_C(st, l, s, hT, hTb)

    def seg_ffn(st, l, s):
        hT = sb("hT", [128, 16, T], BF16, st)
        hTb = Buf("hT")
        src = xT_in[s] if l == 0 else xT_s[s]
        skey = ('xin', s) if l == 0 else ('x', s)
        with ExitStack() as st2:
            norm_pass(st2, l, V_POSTMIX, l, V_PREMLP, src, skey, oT_s[s], xT_s[s], ('x', s), hT, hTb, ('oT', s))
        P.barrier()
        phase_F1(st, l, hT, hTb)

    def seg_final(st, s):
        norm_pass(st, NL - 1, V_POSTMLP, 0, 0, xT_s[s], ('x', s), oT_s[s], outT[s], ('out', s), None, None, ('oT', s))

    ph = phases or "ARSCFGZ"
    for l in range(NL):
        for s in range(NSEQ):
            if 'A' in ph:
                scoped(seg_in, l, s)
            if 'R' in ph:
                scoped(phase_R, l, s)
            if 'S' in ph:
                scoped(phase_S, l)
            if 'C' in ph:
                scoped(seg_mid, l, s)
            if 'F' in ph:
                scoped(seg_ffn, l, s)
            if 'G' in ph:
                scoped(phase_F2, l, s)
            if l == NL - 1 and 'Z' in ph:
                scoped(seg_final, s)
    P.finish()
    P.emit(es)
    es.close()
    return nc


_CACHE = {}


def to_tiles(xs):
    return np.ascontiguousarray(np.transpose(xs, (0, 2, 1)))


def from_tiles(xt):
    return np.transpose(np.asarray(xt), (0, 2, 1))


def kernel(**inputs):
    B, T, _ = inputs['x'].shape
    NL = inputs['w_in'].shape[0]
    NCORES = 8
    NSEQ = B // NCORES
    key = (T, NL, NSEQ)
    if key not in _CACHE:
        _CACHE[key] = build(T, NL, NSEQ)
    nc = _CACHE[key]
    x = np.asarray(inputs['x'], np.float32)
    consts = make_consts()
    vecs = pack_vecs(inputs, NL)
    shared = {
        'consts': consts, 'vecs': vecs,
        'w_in': np.ascontiguousarray(inputs['w_in'], np.float32),
        'w_in_vres': np.ascontiguousarray(inputs['w_in_vres'], np.float32),
        'w_up': np.ascontiguousarray(inputs['w_up'], np.float32),
        'a_up': np.ascontiguousarray(inputs['a_up'], np.float32),
        'g_up': np.ascontiguousarray(inputs['g_up'], np.float32),
        'v_up': np.ascontiguousarray(inputs['v_up'], np.float32),
        'w_out': np.ascontiguousarray(inputs['w_out'], np.float32),
        'w_ff_up': np.ascontiguousarray(inputs['w_ff_up'], np.float32),
        'w_ff_down': np.ascontiguousarray(inputs['w_ff_down'], np.float32),
    }
    in_maps = []
    for c in range(NCORES):
        m = dict(shared)
        m['xT'] = np.ascontiguousarray(np.transpose(x[c * NSEQ:(c + 1) * NSEQ], (0, 2, 1)))
        in_maps.append(m)
    res = run_bass_kernel_spmd(nc, in_maps, core_ids=list(range(NCORES)))
    outs = [np.transpose(r['outT'], (0, 2, 1)) for r in res.results]
    return np.ascontiguousarray(np.concatenate(outs, axis=0)).astype(np.float32)
```
